# Optimizing a Trainium2 kernel written in Bass

```python
import jax
import jax.numpy as jnp
from jax import lax
import numpy as np

D_MODEL = 1024
BATCH = 4
SEQ = 4096
DEPTH = 4

HEAD_DIM = 64
D_RWKV = D_MODEL // 2
RWKV_HEADS = D_RWKV // HEAD_DIM
DECAY_LORA = 64
ICL_LORA = 64
GATE_LORA = 128
RWKV_GN_EPS = 64e-5
D_MOBA = D_MODEL // 2
MOBA_HEADS = D_MOBA // HEAD_DIM
MOBA_BLOCK = 256
MOBA_TOPK = 3
MOBA_Q_CHUNK = 32
D_SSM = D_MODEL // 2
SSM_GROUP = 16
SSM_GROUPS = D_SSM // SSM_GROUP
SSM_STATE = 64
N_BRANCHES = 3
N_EXPERT_GROUPS = 4
EXPERTS_PER_GROUP = 8
N_EXPERTS = N_EXPERT_GROUPS * EXPERTS_PER_GROUP
EXPERT_TOPK = 2
D_EXPERT = D_MODEL // 4
MOE_BLOCK = 512
LN_EPS = 1e-5
DEEPNORM_ALPHA = (2 * DEPTH) ** 0.25
DEEPNORM_BETA = (8 * DEPTH) ** -0.25
NEG_INF = -1e30
RWKV_COLS = 3 * D_RWKV + DECAY_LORA + ICL_LORA + GATE_LORA
MOBA_COLS = 3 * D_MOBA
OFF_MOBA = RWKV_COLS
OFF_SSM = OFF_MOBA + MOBA_COLS
OFF_GATE = OFF_SSM + D_SSM
D_IN_PROJ = OFF_GATE + N_BRANCHES * D_MODEL

kernel_name = 'hybrid_rwkv7_moba_s5_hmoe_deepnorm'


def layer_norm(x, w, b):
    xf = x.astype(jnp.float32)
    mu = jnp.mean(xf, -1, keepdims=True)
    var = jnp.mean(jnp.square(xf - mu), -1, keepdims=True)
    y = (xf - mu) * lax.rsqrt(var + LN_EPS) * w.astype(jnp.float32) + b.astype(jnp.float32)
    return y.astype(x.dtype)


def wkv7_scan(r, w, k, v, a, b):
    Bsz, L, H, N = r.shape

    def step(S, inp):
        r_t, w_t, k_t, v_t, a_t, b_t = inp
        sa = jnp.einsum('bhij,bhj->bhi', S, a_t)
        S = S * w_t[:, :, None, :] + sa[..., None] * b_t[:, :, None, :] + v_t[..., None] * k_t[:, :, None, :]
        return S, jnp.einsum('bhij,bhj->bhi', S, r_t)

    xs = tuple(jnp.moveaxis(t, 1, 0) for t in (r, w, k, v, a, b))
    S0 = jnp.zeros((Bsz, H, N, N), jnp.float32)
    _, y = lax.scan(step, S0, xs)
    return jnp.moveaxis(y, 0, 1)


def rwkv7_mixer(z, mu, w0, w2, a0, a2, g2, k_k, k_a, r_k, ln_w, ln_b):
    f32 = jnp.float32
    Bsz, L, _ = z.shape
    z = z.astype(f32)
    z_prev = jnp.pad(z, ((0, 0), (1, 0), (0, 0)))[:, :-1]
    z = z + (z_prev - z) * mu.astype(f32)
    cuts = np.cumsum([D_RWKV, D_RWKV, D_RWKV, DECAY_LORA, ICL_LORA]).tolist()
    r, k, v, xw, xa, xg = jnp.split(z, cuts, axis=-1)
    w_log = -jax.nn.softplus(-(w0.astype(f32) + jnp.tanh(xw) @ w2.astype(f32))) - 0.5
    decay = jnp.exp(-jnp.exp(w_log))
    a = jax.nn.sigmoid(a0.astype(f32) + xa @ a2.astype(f32))
    g = jax.nn.sigmoid(xg) @ g2.astype(f32)

    def heads(t):
        return t.reshape(Bsz, L, RWKV_HEADS, HEAD_DIM)

    kk = heads(k * k_k.astype(f32))
    kk = kk * lax.rsqrt(jnp.maximum(jnp.sum(kk * kk, -1, keepdims=True), 1e-24))
    k = k * (1.0 + (a - 1.0) * k_a.astype(f32))
    rh, kh, vh, ah = heads(r), heads(k), heads(v), heads(a)
    y = wkv7_scan(rh, heads(decay), kh, vh, -kk, kk * ah)
    mean = jnp.mean(y, -1, keepdims=True)
    var = jnp.mean(jnp.square(y - mean), -1, keepdims=True)
    y = ((y - mean) * lax.rsqrt(var + RWKV_GN_EPS)).reshape(Bsz, L, D_RWKV)
    y = y * ln_w.astype(f32) + ln_b.astype(f32)
    bonus = jnp.sum(rh * kh * r_k.astype(f32), -1, keepdims=True) * vh
    return (y + bonus.reshape(Bsz, L, D_RWKV)) * g


def alibi_slopes(n_heads):
    return 2.0 ** (-8.0 * jnp.arange(1, n_heads + 1, dtype=jnp.float32) / n_heads)


def moba_attention(q, k, v):
    f32 = jnp.float32
    Bsz, L, H, Dh = q.shape
    n_blk = -(-L // MOBA_BLOCK)
    Lp = n_blk * MOBA_BLOCK
    k_sel = min(MOBA_TOPK, n_blk)
    pad = ((0, 0), (0, Lp - L), (0, 0), (0, 0))
    q = jnp.pad(q, pad) * (Dh ** -0.5)
    kbt = jnp.pad(k, pad).reshape(Bsz, n_blk, MOBA_BLOCK, H, Dh).transpose(0, 3, 1, 2, 4)
    vbt = jnp.pad(v, pad).reshape(Bsz, n_blk, MOBA_BLOCK, H, Dh).transpose(0, 3, 1, 2, 4)
    kmean = jnp.mean(kbt.astype(f32), axis=3)
    n_chunk = Lp // MOBA_Q_CHUNK
    qc = q.reshape(Bsz, n_chunk, MOBA_Q_CHUNK, H, Dh).transpose(1, 0, 3, 2, 4)
    slopes = alibi_slopes(H)
    bi = jnp.arange(Bsz)[:, None, None, None]
    hi = jnp.arange(H)[None, :, None, None]
    blk_ids = jnp.arange(n_blk)
    offs = jnp.arange(MOBA_BLOCK)

    def one_chunk(args):
        c, qb = args
        q0 = c * MOBA_Q_CHUNK
        blk = q0 // MOBA_BLOCK
        tpos = q0 + jnp.arange(MOBA_Q_CHUNK)
        gate = jnp.einsum('bhqd,bhnd->bhqn', qb.astype(f32), kmean)
        gate = jnp.where(blk_ids < blk, gate, -jnp.inf)
        _, idx = lax.top_k(gate, k_sel)
        valid = idx < blk
        kg = kbt[bi, hi, idx]
        vg = vbt[bi, hi, idx]
        s_sel = jnp.einsum('bhqd,bhqknd->bhqkn', qb, kg, preferred_element_type=f32)
        spos = idx[..., None] * MOBA_BLOCK + offs
        s_sel = s_sel - slopes[:, None, None, None] * (tpos[:, None, None] - spos).astype(f32)
        s_sel = jnp.where(valid[..., None], s_sel, NEG_INF)
        k_own = lax.dynamic_index_in_dim(kbt, blk, axis=2, keepdims=False)
        v_own = lax.dynamic_index_in_dim(vbt, blk, axis=2, keepdims=False)
        s_own = jnp.einsum('bhqd,bhnd->bhqn', qb, k_own, preferred_element_type=f32)
        dist = tpos[:, None] - (blk * MOBA_BLOCK + offs)[None, :]
        s_own = jnp.where(dist >= 0, s_own - slopes[:, None, None] * dist.astype(f32), NEG_INF)
        s = jnp.concatenate([s_sel.reshape(Bsz, H, MOBA_Q_CHUNK, k_sel * MOBA_BLOCK), s_own], -1)
        p = jax.nn.softmax(s, axis=-1)
        p_sel = p[..., :k_sel * MOBA_BLOCK].reshape(Bsz, H, MOBA_Q_CHUNK, k_sel, MOBA_BLOCK).astype(vg.dtype)
        p_own = p[..., k_sel * MOBA_BLOCK:].astype(v_own.dtype)
        return (jnp.einsum('bhqkn,bhqknd->bhqd', p_sel, vg)
                + jnp.einsum('bhqn,bhnd->bhqd', p_own, v_own))

    out = lax.map(one_chunk, (jnp.arange(n_chunk), qc))
    return out.transpose(1, 0, 3, 2, 4).reshape(Bsz, Lp, H * Dh)[:, :L]


def _complex_affine_combine(e1, e2):
    ar1, ai1, br1, bi1 = e1
    ar2, ai2, br2, bi2 = e2
    return (ar2 * ar1 - ai2 * ai1, ar2 * ai1 + ai2 * ar1,
            ar2 * br1 - ai2 * bi1 + br2, ar2 * bi1 + ai2 * br1 + bi2)


def s5_mixer(u, a_re, a_im, b_re, b_im, c_re, c_im, d, log_dt, glu_w, glu_b):
    f32 = jnp.float32
    Bsz, L, _ = u.shape
    u = u.astype(f32)
    a_re = a_re.astype(f32)
    a_im = a_im.astype(f32)
    dt = jnp.exp(log_dt.astype(f32))[:, None]
    mag = jnp.exp(a_re * dt)
    lam_re = mag * jnp.cos(a_im * dt)
    lam_im = mag * jnp.sin(a_im * dt)
    den = a_re * a_re + a_im * a_im
    nr = lam_re - 1.0
    coef_re = (nr * a_re + lam_im * a_im) / den
    coef_im = (lam_im * a_re - nr * a_im) / den
    b_re = b_re.astype(f32)
    b_im = b_im.astype(f32)
    bb_re = coef_re[..., None] * b_re - coef_im[..., None] * b_im
    bb_im = coef_re[..., None] * b_im + coef_im[..., None] * b_re
    ug = u.reshape(Bsz, L, SSM_GROUPS, SSM_GROUP)
    bu_re = jnp.einsum('blgh,gph->blgp', ug, bb_re)
    bu_im = jnp.einsum('blgh,gph->blgp', ug, bb_im)
    lr = jnp.broadcast_to(lam_re, bu_re.shape)
    li = jnp.broadcast_to(lam_im, bu_re.shape)
    _, _, h_re, h_im = lax.associative_scan(_complex_affine_combine, (lr, li, bu_re, bu_im), axis=1)
    y = (jnp.einsum('blgp,ghp->blgh', h_re, c_re.astype(f32))
         - jnp.einsum('blgp,ghp->blgh', h_im, c_im.astype(f32)))
    y = y.reshape(Bsz, L, D_SSM) + d.astype(f32) * u
    y = jax.nn.gelu(y)
    zg = y @ glu_w.astype(f32) + glu_b.astype(f32)
    return zg[..., :D_SSM] * jax.nn.sigmoid(zg[..., D_SSM:])


def hybrid_mixer(x, w_in, rwkv_mu, rwkv_w0, rwkv_w2, rwkv_a0, rwkv_a2, rwkv_g2, rwkv_k_k, rwkv_k_a,
                 rwkv_r_k, rwkv_ln_w, rwkv_ln_b, ssm_a_re, ssm_a_im, ssm_b_re, ssm_b_im, ssm_c_re,
                 ssm_c_im, ssm_d, ssm_log_dt, ssm_glu_w, ssm_glu_b, w_up_rwkv, w_up_moba, w_up_ssm,
                 gate_b, w_out):
    Bsz, L, _ = x.shape
    proj = x @ w_in
    y_a = rwkv7_mixer(proj[..., :OFF_MOBA], rwkv_mu, rwkv_w0, rwkv_w2, rwkv_a0, rwkv_a2, rwkv_g2,
                      rwkv_k_k, rwkv_k_a, rwkv_r_k, rwkv_ln_w, rwkv_ln_b).astype(x.dtype)
    q, k, v = jnp.split(proj[..., OFF_MOBA:OFF_SSM], 3, axis=-1)

    def heads(t):
        return t.reshape(Bsz, L, MOBA_HEADS, HEAD_DIM)

    y_b = moba_attention(heads(q), heads(k), heads(v))
    y_c = s5_mixer(proj[..., OFF_SSM:OFF_GATE], ssm_a_re, ssm_a_im, ssm_b_re, ssm_b_im, ssm_c_re,
                   ssm_c_im, ssm_d, ssm_log_dt, ssm_glu_w, ssm_glu_b).astype(x.dtype)
    gates = jax.nn.sigmoid(proj[..., OFF_GATE:] + gate_b).reshape(Bsz, L, N_BRANCHES, D_MODEL)
    merged = (gates[..., 0, :] * (y_a @ w_up_rwkv)
              + gates[..., 1, :] * (y_b @ w_up_moba)
              + gates[..., 2, :] * (y_c @ w_up_ssm))
    return merged @ w_out


def hier_moe(x, router_group_w, router_group_b, router_expert_w, router_expert_b,
             expert_w_gate, expert_w_up, expert_w_down):
    f32 = jnp.float32
    Bsz, L, D = x.shape
    T = Bsz * L
    TK = T * EXPERT_TOPK
    xt = x.reshape(T, D)
    xf = xt.astype(f32)
    g_logits = xf @ router_group_w.astype(f32) + router_group_b.astype(f32)
    g_sel = jnp.argmax(g_logits, axis=-1)
    p_group = jnp.take_along_axis(jax.nn.softmax(g_logits, -1), g_sel[:, None], axis=-1)
    e_logits = (xf @ router_expert_w.astype(f32) + router_expert_b.astype(f32)).reshape(
        T, N_EXPERT_GROUPS, EXPERTS_PER_GROUP)
    e_logits = jnp.take_along_axis(e_logits, g_sel[:, None, None], axis=1)[:, 0]
    top_logit, top_local = lax.top_k(e_logits, EXPERT_TOPK)
    weight = p_group * jax.nn.softmax(top_logit, axis=-1)
    expert = g_sel[:, None] * EXPERTS_PER_GROUP + top_local
    flat_e = expert.reshape(TK)
    order = jnp.argsort(flat_e)
    e_sorted = flat_e[order]
    tok_sorted = order // EXPERT_TOPK
    counts = jnp.bincount(flat_e, length=N_EXPERTS)
    starts = jnp.cumsum(counts) - counts
    padded = (counts + MOE_BLOCK - 1) // MOE_BLOCK * MOE_BLOCK
    p_ends = jnp.cumsum(padded)
    p_starts = p_ends - padded
    dest = p_starts[e_sorted] + jnp.arange(TK) - starts[e_sorted]
    n_blocks = -(-TK // MOE_BLOCK) + N_EXPERTS
    buf = jnp.zeros((n_blocks * MOE_BLOCK, D), x.dtype).at[dest].set(xt[tok_sorted])
    blk_e = jnp.minimum(jnp.searchsorted(p_ends, jnp.arange(n_blocks) * MOE_BLOCK, side='right'),
                        N_EXPERTS - 1)
    xb = buf.reshape(n_blocks, MOE_BLOCK, D)
    hid = (jax.nn.silu(jnp.einsum('nbd,ndf->nbf', xb, expert_w_gate[blk_e]))
           * jnp.einsum('nbd,ndf->nbf', xb, expert_w_up[blk_e]))
    yb = jnp.einsum('nbf,nfd->nbd', hid, expert_w_down[blk_e]).reshape(n_blocks * MOE_BLOCK, D)
    w_sorted = weight.reshape(TK)[order].astype(x.dtype)
    y = jnp.zeros((T, D), x.dtype).at[tok_sorted].add(yb[dest] * w_sorted[:, None])
    return y.reshape(Bsz, L, D)


def setup_inputs(seed: int = 0) -> dict:
    key = jax.random.key(seed)
    keys = iter(jax.random.split(key, 48))
    f32 = jnp.float32

    def nrm(shape, scale):
        return scale * jax.random.normal(next(keys), shape, f32)

    def near(shape, center, spread):
        return center + spread * jax.random.normal(next(keys), shape, f32)

    Ld = DEPTH
    ratio = jnp.arange(D_RWKV, dtype=f32) / (D_RWKV - 1)
    w0_base = -7.0 + 5.0 * ratio ** 0.85 + 0.5
    a_im_base = jnp.pi * jnp.arange(SSM_STATE, dtype=f32)
    return {
        'x': nrm((BATCH, SEQ, D_MODEL), 1.0),
        'w_in': nrm((Ld, D_MODEL, D_IN_PROJ), D_MODEL ** -0.5),
        'rwkv_mu': jax.random.uniform(next(keys), (Ld, RWKV_COLS), f32),
        'rwkv_w0': w0_base + nrm((Ld, D_RWKV), 0.1),
        'rwkv_w2': nrm((Ld, DECAY_LORA, D_RWKV), 0.5 * DECAY_LORA ** -0.5),
        'rwkv_a0': nrm((Ld, D_RWKV), 0.1),
        'rwkv_a2': nrm((Ld, ICL_LORA, D_RWKV), ICL_LORA ** -0.5),
        'rwkv_g2': nrm((Ld, GATE_LORA, D_RWKV), GATE_LORA ** -0.5),
        'rwkv_k_k': near((Ld, D_RWKV), 0.85, 0.05),
        'rwkv_k_a': near((Ld, D_RWKV), 1.0, 0.05),
        'rwkv_r_k': nrm((Ld, RWKV_HEADS, HEAD_DIM), 0.1),
        'rwkv_ln_w': near((Ld, D_RWKV), 1.0, 0.05),
        'rwkv_ln_b': nrm((Ld, D_RWKV), 0.02),
        'ssm_a_re': near((Ld, SSM_GROUPS, SSM_STATE), -0.5, 0.01),
        'ssm_a_im': jnp.broadcast_to(a_im_base, (Ld, SSM_GROUPS, SSM_STATE)) + nrm((Ld, SSM_GROUPS, SSM_STATE), 0.01),
        'ssm_b_re': nrm((Ld, SSM_GROUPS, SSM_STATE, SSM_GROUP), (2 * SSM_GROUP) ** -0.5),
        'ssm_b_im': nrm((Ld, SSM_GROUPS, SSM_STATE, SSM_GROUP), (2 * SSM_GROUP) ** -0.5),
        'ssm_c_re': nrm((Ld, SSM_GROUPS, SSM_GROUP, SSM_STATE), 0.5),
        'ssm_c_im': nrm((Ld, SSM_GROUPS, SSM_GROUP, SSM_STATE), 0.5),
        'ssm_d': nrm((Ld, D_SSM), 1.0),
        'ssm_log_dt': jax.random.uniform(next(keys), (Ld, SSM_GROUPS), f32,
                                         minval=float(np.log(1e-3)), maxval=float(np.log(1e-1))),
        'ssm_glu_w': nrm((Ld, D_SSM, 2 * D_SSM), D_SSM ** -0.5),
        'ssm_glu_b': nrm((Ld, 2 * D_SSM), 0.02),
        'w_up_rwkv': nrm((Ld, D_RWKV, D_MODEL), D_RWKV ** -0.5),
        'w_up_moba': nrm((Ld, D_MOBA, D_MODEL), D_MOBA ** -0.5),
        'w_up_ssm': nrm((Ld, D_SSM, D_MODEL), D_SSM ** -0.5),
        'gate_b': nrm((Ld, N_BRANCHES * D_MODEL), 0.1),
        'w_out': nrm((Ld, D_MODEL, D_MODEL), DEEPNORM_BETA * D_MODEL ** -0.5),
        'ln1_w': near((Ld, D_MODEL), 1.0, 0.05),
        'ln1_b': nrm((Ld, D_MODEL), 0.02),
        'router_group_w': nrm((Ld, D_MODEL, N_EXPERT_GROUPS), D_MODEL ** -0.5),
        'router_group_b': nrm((Ld, N_EXPERT_GROUPS), 0.01),
        'router_expert_w': nrm((Ld, D_MODEL, N_EXPERTS), D_MODEL ** -0.5),
        'router_expert_b': nrm((Ld, N_EXPERTS), 0.01),
        'expert_w_gate': nrm((Ld, N_EXPERTS, D_MODEL, D_EXPERT), D_MODEL ** -0.5),
        'expert_w_up': nrm((Ld, N_EXPERTS, D_MODEL, D_EXPERT), D_MODEL ** -0.5),
        'expert_w_down': nrm((Ld, N_EXPERTS, D_EXPERT, D_MODEL), DEEPNORM_BETA * D_EXPERT ** -0.5),
        'ln2_w': near((Ld, D_MODEL), 1.0, 0.05),
        'ln2_b': nrm((Ld, D_MODEL), 0.02),
    }


def reference(x, w_in, rwkv_mu, rwkv_w0, rwkv_w2, rwkv_a0, rwkv_a2, rwkv_g2, rwkv_k_k, rwkv_k_a,
              rwkv_r_k, rwkv_ln_w, rwkv_ln_b, ssm_a_re, ssm_a_im, ssm_b_re, ssm_b_im, ssm_c_re,
              ssm_c_im, ssm_d, ssm_log_dt, ssm_glu_w, ssm_glu_b, w_up_rwkv, w_up_moba, w_up_ssm,
              gate_b, w_out, ln1_w, ln1_b, router_group_w, router_group_b, router_expert_w,
              router_expert_b, expert_w_gate, expert_w_up, expert_w_down, ln2_w, ln2_b):
    for l in range(DEPTH):
        h = hybrid_mixer(x, w_in[l], rwkv_mu[l], rwkv_w0[l], rwkv_w2[l], rwkv_a0[l], rwkv_a2[l],
                         rwkv_g2[l], rwkv_k_k[l], rwkv_k_a[l], rwkv_r_k[l], rwkv_ln_w[l], rwkv_ln_b[l],
                         ssm_a_re[l], ssm_a_im[l], ssm_b_re[l], ssm_b_im[l], ssm_c_re[l], ssm_c_im[l],
                         ssm_d[l], ssm_log_dt[l], ssm_glu_w[l], ssm_glu_b[l], w_up_rwkv[l],
                         w_up_moba[l], w_up_ssm[l], gate_b[l], w_out[l])
        x = layer_norm(DEEPNORM_ALPHA * x + h, ln1_w[l], ln1_b[l])
        h = hier_moe(x, router_group_w[l], router_group_b[l], router_expert_w[l], router_expert_b[l],
                     expert_w_gate[l], expert_w_up[l], expert_w_down[l])
        x = layer_norm(DEEPNORM_ALPHA * x + h, ln2_w[l], ln2_b[l])
    return x
```

```python
import math
import numpy as np
from contextlib import ExitStack
import concourse.bass as bass
import concourse.mybir as mybir
from concourse.bass_utils import run_bass_kernel_spmd

F32 = mybir.dt.float32
BF16 = mybir.dt.bfloat16
I32 = mybir.dt.int32
ALU = mybir.AluOpType
AF = mybir.ActivationFunctionType
AX = mybir.AxisListType

COMPUTE = ("pe", "act", "dve", "pool")
NSLOT = 12

L_SEQ = 4096
DM = 1024
DEPTH = 4
ALPHA = (2 * DEPTH) ** 0.25
DIN = 6912
TB = 128
NBLK = L_SEQ // TB


def _k(x):
    if isinstance(x, tuple):
        ap, key = x
        if isinstance(key, int):
            key = "%s#%d" % (ap.name, key)
        return ap, key
    return x, x.name


class Prog:
    def __init__(self, nc, stack):
        self.nc = nc
        self.stack = stack
        self.eng = {"pe": nc.tensor, "act": nc.scalar, "dve": nc.vector,
                    "pool": nc.gpsimd, "sp": nc.sync}
        self.ins = []
        self.last_w = {}
        self.readers = {}
        self.last_barrier = None
        self.since_barrier = {}
        self.dmas_since = []
        self.psn = 0
        self.subs = {}
        self.bregs = {}

    def sb(self, name, shape, dt=F32, stack=None):
        self.nsb = getattr(self, "nsb", 0) + 1
        name = "%s_t%d" % (name, self.nsb)
        return (stack or self.stack).enter_context(self.nc.sbuf_tensor(name, list(shape), dt))

    def ps(self, name, shape, dt=F32, stack=None):
        return (stack or self.stack).enter_context(self.nc.psum_tensor(name, list(shape), dt))

    def dram(self, name, shape, dt=F32, kind="Internal"):
        return self.nc.dram_tensor(name, list(shape), dt, kind=kind).ap()

    def declare(self, tile, n):
        self.subs[tile.name] = ["%s#%d" % (tile.name, i) for i in range(n)]
        return tile

    def _expand(self, keys):
        out = []
        for k in keys:
            if k in self.subs:
                out.extend(self.subs[k])
            else:
                out.append(k)
        return out

    def _rec(self, eng, fn, reads, writes, dma):
        reads = self._expand(reads)
        writes = self._expand(writes)
        i = len(self.ins)
        deps = set()
        for k in reads:
            w = self.last_w.get(k)
            if w is not None:
                deps.add(w)
        for k in writes:
            w = self.last_w.get(k)
            if w is not None:
                deps.add(w)
            for r in self.readers.get(k, ()):
                deps.add(r)
        if self.last_barrier is not None:
            deps.add(self.last_barrier)
        deps.discard(i)
        for k in reads:
            self.readers.setdefault(k, []).append(i)
        for k in writes:
            self.last_w[k] = i
            self.readers[k] = []
        self.ins.append(dict(eng=eng, fn=fn, deps=deps, dma=dma))
        if dma:
            self.dmas_since.append(i)
        else:
            self.since_barrier[eng] = i
        return i

    def op(self, eng, fn, reads=(), writes=()):
        return self._rec(eng, fn, tuple(reads), tuple(writes), False)

    def barrier(self, scratch):
        deps = set(self.since_barrier.values()) | set(self.dmas_since)
        if self.last_barrier is not None:
            deps.add(self.last_barrier)
        i = len(self.ins)
        self.ins.append(dict(eng="dve", fn=lambda e: e.memset(scratch, 0.0), deps=deps, dma=False, barrier=True))
        self.last_barrier = i
        self.since_barrier = {"dve": i}
        self.dmas_since = []
        self.last_w = {}
        self.readers = {}
        return i

    def dma(self, q, out, in_, **kw):
        o, ok = _k(out)
        a, ak = _k(in_)
        return self._rec(q, lambda e: e.dma_start(out=o, in_=a, **kw), (ak,), (ok,), True)

    def dma_fn(self, q, fn, reads=(), writes=()):
        return self._rec(q, fn, tuple(reads), tuple(writes), True)

    def gather(self, out, src, idx, bound=None):
        o, ok = _k(out)
        s, sk = _k(src)
        ix, ik = _k(idx)
        rk = (sk, ik)
        if bound is None:
            return self._rec("pool", lambda e: e.indirect_dma_start(
                out=o, out_offset=None, in_=s, in_offset=bass.IndirectOffsetOnAxis(ap=ix, axis=0)),
                rk, (ok,), True)
        rk = (sk, ik, ok)

        def fn(e):
            if bound not in self.bregs:
                self.bregs[bound] = e.to_reg(bound)
            return e.indirect_dma_start(out=o, out_offset=None, in_=s,
                                        in_offset=bass.IndirectOffsetOnAxis(ap=ix, axis=0),
                                        bounds_check=self.bregs[bound], oob_is_err=False)
        return self._rec("pool", fn, rk, (ok,), True)

    def scatter(self, dst, src, idx):
        o, ok = _k(dst)
        s, sk = _k(src)
        ix, ik = _k(idx)
        return self._rec("pool", lambda e: e.indirect_dma_start(
            out=o, out_offset=bass.IndirectOffsetOnAxis(ap=ix, axis=0), in_=s, in_offset=None),
            (sk, ik), (ok,), True)

    def mm(self, out, lhsT, rhs, start=True, stop=True, xr=(), **kw):
        o, ok = _k(out)
        a, ak = _k(lhsT)
        b, bk = _k(rhs)
        return self.op("pe", lambda e: e.matmul(o, lhsT=a, rhs=b, start=start, stop=stop, **kw),
                       (ak, bk) + tuple(xr), (ok,))

    def tr(self, out, in_, ident):
        o, ok = _k(out)
        a, ak = _k(in_)
        b, bk = _k(ident)
        return self.op("pe", lambda e: e.transpose(out=o, in_=a, identity=b), (ak, bk), (ok,))

    def act(self, out, in_, func, bias=None, scale=None, accum=None, eng="act"):
        o, ok = _k(out)
        a, ak = _k(in_)
        rk = [ak]
        kw = {}
        if bias is not None:
            if isinstance(bias, (int, float)):
                kw["bias"] = float(bias)
            else:
                b, bk = _k(bias)
                kw["bias"] = b
                rk.append(bk)
        if scale is not None:
            if isinstance(scale, (int, float)):
                kw["scale"] = float(scale)
            else:
                s, sk = _k(scale)
                kw["scale"] = s
                rk.append(sk)
        wk = [ok]
        if accum is not None:
            c, ck = _k(accum)
            kw["accum_out"] = c
            wk.append(ck)
        return self.op("act", lambda e: e.activation(out=o, in_=a, func=func, **kw), rk, wk)

    def tt(self, eng, out, a, b, op):
        o, ok = _k(out)
        x, xk = _k(a)
        y, yk = _k(b)
        return self.op(eng, lambda e: e.tensor_tensor(out=o, in0=x, in1=y, op=op), (xk, yk), (ok,))

    def ts(self, eng, out, a, s1, op0, s2=None, op1=None, accum=None):
        o, ok = _k(out)
        x, xk = _k(a)
        rk = [xk]

        def sc(s):
            if s is None or isinstance(s, (int, float)):
                return None if s is None else float(s)
            p, pk = _k(s)
            rk.append(pk)
            return p
        v1 = sc(s1)
        v2 = sc(s2)
        kw = {}
        wk = [ok]
        if op1 is not None:
            kw["op1"] = op1
        if accum is not None:
            c, ck = _k(accum)
            kw["accum_out"] = c
            wk.append(ck)
        return self.op(eng, lambda e: e.tensor_scalar(out=o, in0=x, scalar1=v1, scalar2=v2, op0=op0, **kw),
                       rk, wk)

    def stt(self, eng, out, in0, scalar, in1, op0, op1):
        o, ok = _k(out)
        x, xk = _k(in0)
        y, yk = _k(in1)
        rk = [xk, yk]
        if isinstance(scalar, (int, float)):
            sv = float(scalar)
        else:
            sv, sk = _k(scalar)
            rk.append(sk)
        return self.op(eng, lambda e: e.scalar_tensor_tensor(out=o, in0=x, scalar=sv, in1=y, op0=op0, op1=op1),
                       rk, (ok,))

    def cp(self, eng, out, in_):
        o, ok = _k(out)
        a, ak = _k(in_)
        if eng == "act":
            return self.op("act", lambda e: e.copy(out=o, in_=a), (ak,), (ok,))
        return self.op(eng, lambda e: e.tensor_copy(out=o, in_=a), (ak,), (ok,))

    def memset(self, eng, out, val):
        o, ok = _k(out)
        return self.op(eng, lambda e: e.memset(o, val), (), (ok,))

    def scan(self, out, d0, d1, init, op0=ALU.mult, op1=ALU.add):
        o, ok = _k(out)
        a, ak = _k(d0)
        b, bk = _k(d1)
        rk = [ak, bk]
        if isinstance(init, (int, float)):
            iv = float(init)
        else:
            iv, ik = _k(init)
            rk.append(ik)
        return self.op("dve", lambda e: e.tensor_tensor_scan(out=o, data0=a, data1=b, initial=iv, op0=op0, op1=op1),
                       rk, (ok,))

    def finish(self, final_wait_eng="sp"):
        nc = self.nc
        ins = self.ins
        n = len(ins)

        def skip(src, it):
            return src["eng"] == "pe" and it["eng"] == "pe" and not src["dma"] and not it["dma"]
        need = [False] * n
        for i, it in enumerate(ins):
            for d in it["deps"]:
                if not skip(ins[d], it):
                    need[d] = True
        sems = {e: self.stack.enter_context(nc.semaphore("s_" + e)) for e in COMPUTE}
        qs = sorted({it["eng"] for it in ins if it["dma"]})
        dsem = {q: [self.stack.enter_context(nc.semaphore("d_%s_%d" % (q, j)))
                    for j in range(NSLOT)] for q in qs}
        cnt = {e: 0 for e in COMPUTE}
        dcnt = {q: 0 for q in qs}
        slot_uses = {q: [0] * NSLOT for q in qs}
        sig = [None] * n
        waited = {}
        nwait = 0
        epoch = 0
        for i, it in enumerate(ins):
            e = it["eng"]
            eo = self.eng[e]
            want = {}
            for d in it["deps"]:
                src = ins[d]
                if skip(src, it):
                    continue
                s, v = sig[d]
                key = id(s)
                if key not in want or want[key][1] < v:
                    want[key] = (s, v)
            if it["dma"]:
                j = dcnt[e] % NSLOT
                if slot_uses[e][j] > 0:
                    s = dsem[e][j]
                    v = 16 * slot_uses[e][j]
                    key = id(s)
                    if key not in want or want[key][1] < v:
                        want[key] = (s, v)
            for key, (s, v) in want.items():
                wk = (e, key)
                if waited.get(wk, -1) >= v:
                    continue
                eo.wait_ge(s, v)
                nwait += 1
                waited[wk] = v
            bi = it["fn"](eo)
            if it["dma"]:
                j = dcnt[e] % NSLOT
                slot_uses[e][j] += 1
                dcnt[e] += 1
                bi.then_inc(dsem[e][j], 16)
                sig[i] = (dsem[e][j], 16 * slot_uses[e][j])
            elif need[i]:
                cnt[e] += 1
                bi.then_inc(sems[e], 1)
                sig[i] = (sems[e], cnt[e])
            if it.get("barrier"):
                epoch += 1
                sems = {e2: self.stack.enter_context(nc.semaphore("s%d_%s" % (epoch, e2))) for e2 in COMPUTE}
                cnt = {e2: 0 for e2 in COMPUTE}
        fe = self.eng[final_wait_eng]
        for q in qs:
            for j in range(NSLOT):
                if slot_uses[q][j] > 0:
                    fe.wait_ge(dsem[q][j], 16 * slot_uses[q][j])
        self.stats = dict(n=n, cnt=dict(cnt), dcnt=dict(dcnt), nwait=nwait)
        return self.stats


def fm(a, nt):
    return np.ascontiguousarray(np.asarray(a).reshape(nt, 128).T)


def prep_shared(inp):
    f = np.float32
    d = {}
    d["w_in"] = np.ascontiguousarray(inp["w_in"], dtype=f)
    d["mu_fm"] = np.stack([fm(inp["rwkv_mu"][l], 14) for l in range(DEPTH)]).astype(f)
    d["gate_b_fm"] = np.stack([fm(inp["gate_b"][l], 24) for l in range(DEPTH)]).astype(f)
    d["ident"] = np.eye(128, dtype=f)
    are, aim, ldt = inp["ssm_a_re"], inp["ssm_a_im"], inp["ssm_log_dt"]
    s5fm = np.zeros((DEPTH, 128, 3, 16), f)
    for st in range(16):
        for gg in range(2):
            g = 2 * st + gg
            s5fm[:, gg * 64:(gg + 1) * 64, 0, st] = are[:, g, :]
            s5fm[:, gg * 64:(gg + 1) * 64, 1, st] = aim[:, g, :]
            s5fm[:, gg * 64:(gg + 1) * 64, 2, st] = ldt[:, g, None]
    d["s5fm"] = s5fm
    bre, bim = inp["ssm_b_re"], inp["ssm_b_im"]
    s5row = np.zeros((DEPTH, 128, 4, 5, 64), f)
    for kc in range(4):
        for g8 in range(8):
            g = 8 * kc + g8
            rs = slice(g8 * 16, g8 * 16 + 16)
            s5row[:, rs, kc, 0, :] = are[:, g, None, :]
            s5row[:, rs, kc, 1, :] = aim[:, g, None, :]
            s5row[:, rs, kc, 2, :] = ldt[:, g, None, None]
            s5row[:, rs, kc, 3, :] = bre[:, g].transpose(0, 2, 1)
            s5row[:, rs, kc, 4, :] = bim[:, g].transpose(0, 2, 1)
    d["s5row"] = s5row
    cre, cim = inp["ssm_c_re"], inp["ssm_c_im"]
    s5c = np.zeros((DEPTH, 128, 2, 16, 16), f)
    for st in range(16):
        for gg in range(2):
            g = 2 * st + gg
            s5c[:, gg * 64:(gg + 1) * 64, 0, st, :] = cre[:, g].transpose(0, 2, 1)
            s5c[:, gg * 64:(gg + 1) * 64, 1, st, :] = cim[:, g].transpose(0, 2, 1)
    d["s5c"] = s5c
    rowmask = np.zeros((128, 8), f)
    for g8 in range(8):
        rowmask[g8 * 16:(g8 + 1) * 16, g8] = 1
    halfmask = np.zeros((128, 2), f)
    halfmask[:64, 0] = 1
    halfmask[64:, 1] = 1
    d["rowmask"] = rowmask
    d["halfmask"] = halfmask
    d["tvals"] = np.tile(np.arange(1, 129, dtype=f)[None, :], (128, 1))
    d["ssm_d_fm"] = np.stack([fm(inp["ssm_d"][l], 4) for l in range(DEPTH)]).astype(f)
    d["glu_b_fm"] = np.stack([fm(inp["ssm_glu_b"][l], 8) for l in range(DEPTH)]).astype(f)
    d["glu_w"] = np.ascontiguousarray(inp["ssm_glu_w"], dtype=f)
    import ml_dtypes
    slopes = (2.0 ** (-(np.arange(1, 9, dtype=np.float64)))).astype(f)
    aj = slopes[:, None] * np.arange(256, dtype=f)[None, :]
    hi = aj.astype(ml_dtypes.bfloat16).astype(f)
    lo = (aj - hi).astype(ml_dtypes.bfloat16).astype(f)
    d["alibi_hl"] = np.stack([hi, lo]).astype(f)
    p = np.arange(128, dtype=f)[:, None, None]
    n = np.arange(16, dtype=f)[None, None, :]
    d["mb_t1"] = (-slopes[None, :, None] * (p - 256.0 * n)).astype(f)
    d["mb_slopes"] = np.tile(slopes[None, :], (128, 1)).astype(f)
    tri = np.zeros((128, 128), f)
    tri[np.arange(128)[:, None] < np.arange(128)[None, :]] = -30000.0
    d["mb_tri"] = tri
    rv = np.zeros((DEPTH, 128, 5, 4), f)
    for l in range(DEPTH):
        rv[l, :, 0, :] = fm(inp["rwkv_w0"][l], 4)
        rv[l, :, 1, :] = fm(inp["rwkv_a0"][l], 4)
        rv[l, :, 2, :] = fm(inp["rwkv_k_k"][l], 4)
        rv[l, :, 3, :] = fm(inp["rwkv_k_a"][l], 4)
        rv[l, :, 4, :] = fm(inp["rwkv_r_k"][l].reshape(512), 4)
    d["rw_vecs"] = rv
    d["rw_w2a2"] = np.concatenate([inp["rwkv_w2"], inp["rwkv_a2"]], axis=1).astype(f)
    d["rw_g2"] = np.ascontiguousarray(inp["rwkv_g2"], dtype=f)
    d["rw_ln"] = np.stack([np.tile(inp["rwkv_ln_w"][:, None, :], (1, 64, 1)),
                           np.tile(inp["rwkv_ln_b"][:, None, :], (1, 64, 1))], axis=2).astype(f)
    tt_ = np.arange(64)
    ml = (tt_[None, :] < tt_[:, None]).astype(f)
    mu_ = (tt_[None, :] > tt_[:, None]).astype(f)
    mui = (tt_[None, :] >= tt_[:, None]).astype(f)
    d["rw_masks"] = np.stack([ml, mu_, mui], axis=1).astype(f)
    bo = np.zeros((128, 128), f)
    bo[:64, :64] = 1
    bo[64:, 64:] = 1
    d["rw_bo"] = bo
    d["w_up"] = np.stack([inp["w_up_rwkv"], inp["w_up_moba"], inp["w_up_ssm"]], axis=1).astype(f)
    d["w_out"] = np.ascontiguousarray(inp["w_out"], dtype=f)
    d["ln1_rep"] = np.stack([np.tile(inp["ln1_w"][:, None, :], (1, 128, 1)),
                             np.tile(inp["ln1_b"][:, None, :], (1, 128, 1))], axis=1).astype(f)
    rw = np.concatenate([inp["router_group_w"], inp["router_expert_w"]], axis=2).astype(f)
    d["moe_rw"] = np.ascontiguousarray(rw.reshape(DEPTH, 8, 128, 36).transpose(0, 2, 1, 3))
    rb = np.concatenate([inp["router_group_b"], inp["router_expert_b"]], axis=1).astype(f)
    d["moe_rb"] = np.ascontiguousarray(np.tile(rb[:, None, :], (1, 128, 1)))
    wg = inp["expert_w_gate"].reshape(DEPTH, 32, 8, 128, 256).transpose(0, 1, 3, 2, 4)
    wu = inp["expert_w_up"].reshape(DEPTH, 32, 8, 128, 256).transpose(0, 1, 3, 2, 4)
    d["moe_wgu"] = np.ascontiguousarray(np.stack([wg, wu], axis=3).reshape(DEPTH, 32 * 128, 4096), dtype=f)
    wd = inp["expert_w_down"].reshape(DEPTH, 32, 2, 128, 1024).transpose(0, 1, 3, 2, 4)
    d["moe_wd"] = np.ascontiguousarray(wd.reshape(DEPTH, 32 * 128, 2048), dtype=f)
    tri_s = (np.arange(128)[:, None] < np.arange(128)[None, :]).astype(f)
    d["moe_tri"] = tri_s
    d["moe_thr"] = np.tile((128.0 * np.arange(64, dtype=f))[None, :], (128, 1))
    d["moe_nv"] = np.tile((128.0 * np.arange(96, dtype=f))[None, :], (128, 1))
    d["moe_pv"] = np.arange(128, dtype=f)[:, None].copy()
    d["ln2_rep"] = np.stack([np.tile(inp["ln2_w"][:, None, :], (1, 128, 1)),
                             np.tile(inp["ln2_b"][:, None, :], (1, 128, 1))], axis=1).astype(f)
    return d


DBG = {}
RWCUT = 9
SKIP = set()
NPSF = 6
PI = math.pi

IN_SHAPES = {
    "x": [L_SEQ, DM], "w_in": [DEPTH, DM, DIN], "mu_fm": [DEPTH, 128, 14], "gate_b_fm": [DEPTH, 128, 24],
    "ident": [128, 128], "s5fm": [DEPTH, 128, 3, 16], "s5row": [DEPTH, 128, 4, 5, 64],
    "s5c": [DEPTH, 128, 2, 16, 16], "rowmask": [128, 8], "halfmask": [128, 2], "tvals": [128, 128],
    "ssm_d_fm": [DEPTH, 128, 4], "glu_b_fm": [DEPTH, 128, 8], "glu_w": [DEPTH, 512, 1024],
    "alibi_hl": [2, 8, 256], "mb_t1": [128, 8, 16], "mb_slopes": [128, 8], "mb_tri": [128, 128],
    "rw_vecs": [DEPTH, 128, 5, 4], "rw_w2a2": [DEPTH, 128, 512], "rw_g2": [DEPTH, 128, 512],
    "rw_ln": [DEPTH, 64, 2, 512], "rw_masks": [64, 3, 64], "rw_bo": [128, 128],
    "w_up": [DEPTH, 3, 512, 1024], "w_out": [DEPTH, 1024, 1024],
    "ln1_rep": [DEPTH, 2, 128, 1024], "ln2_rep": [DEPTH, 2, 128, 1024],
    "moe_rw": [DEPTH, 128, 8, 36], "moe_rb": [DEPTH, 128, 36], "moe_wgu": [DEPTH, 4096, 4096],
    "moe_wd": [DEPTH, 4096, 2048], "moe_tri": [128, 128], "moe_thr": [128, 64], "moe_nv": [128, 96],
    "moe_pv": [128, 1],
}


def build(nc, n_layers=DEPTH, debug=False, nblk=NBLK, dbg_blk=1):
    D = {k: nc.dram_tensor(k, v, F32, kind="ExternalInput").ap() for k, v in IN_SHAPES.items()}
    OUT = nc.dram_tensor("out", [L_SEQ, DM], F32, kind="ExternalOutput").ap()
    dbg = {}
    if debug:
        for nm, shp in DBG.items():
            dbg[nm] = nc.dram_tensor("dbg_" + nm, list(shp), F32, kind="ExternalOutput").ap()

    with ExitStack() as st:
        P = Prog(nc, st)
        XTd = P.dram("xtd", [128, 8, L_SEQ + 1], BF16)
        identf = P.sb("identf", [128, 128], F32)
        identb = P.sb("identb", [128, 128], BF16)
        rowmask = P.sb("rowmask", [128, 8], F32)
        halfmask = P.sb("halfmask", [128, 2], F32)
        tvals = P.sb("tvals", [128, 128], F32)
        bar = P.sb("bar", [128, 1], F32)
        PSF = [P.ps("psf%d" % i, [128, 512], F32) for i in range(NPSF)]
        PSB = [P.ps("psb%d" % i, [128, 1024], BF16) for i in range(2)]

        def nps():
            t = PSF[P.psn % 4]
            P.psn += 1
            return t

        P.dma("sp", identf[:], D["ident"][:, :])
        P.dma("sp", rowmask[:], D["rowmask"][:, :])
        P.dma("sp", halfmask[:], D["halfmask"][:, :])
        P.dma("sp", tvals[:], D["tvals"][:, :])
        P.cp("dve", identb[:], identf[:])
        ALR = P.sb("ALR", [128, 8, 256], BF16)
        ones2 = P.sb("ones2", [128, 128], BF16)
        mbT1 = P.sb("mbT1", [128, 8, 16], F32)
        mbSl = P.sb("mbSl", [128, 8], F32)
        trib = P.sb("trib", [128, 128], BF16)
        P.memset("pool", ones2[:], 1.0)
        P.dma("sp", mbT1[:], D["mb_t1"][:, :, :])
        P.dma("sp", mbSl[:], D["mb_slopes"][:, :])
        rwmask = P.sb("rwmask", [64, 3, 64], F32)
        BOb = P.sb("BOb", [128, 128], BF16)
        ones64 = P.sb("ones64", [128, 64], F32)
        P.dma("sp", rwmask[:], D["rw_masks"][:, :, :])
        P.memset("pool", ones64[:], 1.0)
        KTd = P.dram("ktd", [16, 128, 4, 256], BF16)
        Vd = P.dram("vd", [16, 128, 2, 8 * 65], BF16)

        WINB = P.dram("winb", [DEPTH, DM, DIN], BF16)
        GLUB = P.dram("glub", [DEPTH, 512, 1024], BF16)
        UPB = P.dram("upb", [DEPTH, 3, 512, 1024], BF16)
        WOB = P.dram("wob", [DEPTH, 1024, 1024], BF16)
        WGUB = [P.dram("wgub%d" % i, [4096, 4096], BF16) for i in range(DEPTH)]
        WDB = [P.dram("wdb%d" % i, [4096, 2048], BF16) for i in range(DEPTH)]
        XS = P.dram("xsort", [96 * 128, DM], BF16)
        YBd = P.dram("ybd", [96 * 128, DM], F32)
        X1 = P.dram("x1s", [L_SEQ, DM], F32)
        X2 = P.dram("x2s", [L_SEQ, DM], F32)
        for l in range(n_layers):
            for r0 in range(0, DM, 256):
                P.dma("pool", (WINB[l, r0:r0 + 256, :], "winb%d" % l), (D["w_in"][l, r0:r0 + 256, :], "w_in"))
            P.dma("pool", (GLUB[l], "glub%d" % l), (D["glu_w"][l], "glu_w"))
            for br in range(3):
                P.dma("pool", (UPB[l, br], "upb%d" % l), (D["w_up"][l, br], "w_up"))
            P.dma("pool", (WOB[l], "wob%d" % l), (D["w_out"][l], "w_out"))
            for r0 in range(0, 4096, 512):
                P.dma("pool", (WGUB[l][r0:r0 + 512, :], "wgub%d" % l), (D["moe_wgu"][l, r0:r0 + 512, :], "moe_wgu"))
            for r0 in range(0, 4096, 1024):
                P.dma("pool", (WDB[l][r0:r0 + 1024, :], "wdb%d" % l), (D["moe_wd"][l, r0:r0 + 1024, :], "moe_wd"))

        with ExitStack() as s0:
            alr32 = P.sb("alr32", [128, 8, 256], F32, s0)
            tri32 = P.sb("tri32", [128, 128], F32, s0)
            bo32 = P.sb("bo32", [128, 128], F32, s0)
            P.memset("pool", alr32[:], 0.0)
            P.dma("sp", alr32[0:2], D["alibi_hl"][:, :, :])
            P.dma("sp", alr32[64:66], D["alibi_hl"][:, :, :])
            P.cp("dve", ALR[:], alr32[:])
            P.dma("sp", tri32[:], D["mb_tri"][:, :])
            P.cp("dve", trib[:], tri32[:])
            P.dma("sp", bo32[:], D["rw_bo"][:, :])
            P.cp("dve", BOb[:], bo32[:])
            zt = P.sb("zt", [128, DM], BF16, s0)
            P.memset("pool", zt[:], 0.0)
            for n in range(2 * nblk + 32):
                P.dma("sp", (XS[n * 128:(n + 1) * 128, :], "xs"), zt[:])
            xin = [P.sb("xin%d" % i, [128, DM], F32, s0) for i in range(2)]
            xinb = [P.sb("xinb%d" % i, [128, DM], BF16, s0) for i in range(2)]
            xz = P.sb("xz", [128, 8, 1], BF16, s0)
            xst = [P.sb("xst%d" % i, [128, 8, 128], BF16, s0) for i in range(2)]
            P.memset("pool", xz[:], 0.0)
            P.dma("sp", (XTd[:, :, 0:1], "XTz"), xz[:], allow_slow_non_contiguous=True)
            for ti in range(0 if 'xload' in SKIP else L_SEQ // 128):
                h2 = ti % 2
                xi, xb, pb = xin[h2], xinb[h2], PSB[h2]
                P.dma("sp", xi[:], (D["x"][ti * 128:(ti + 1) * 128, :], "x"))
                P.cp("act" if h2 else "dve", xb[:], xi[:])
                for kc in range(8):
                    P.tr(pb[:, kc * 128:(kc + 1) * 128], xb[:, kc * 128:(kc + 1) * 128], identb[:])
                P.cp("dve" if h2 else "act", xst[h2][:], pb[:].rearrange("p (k t) -> p k t", k=8))
                P.dma("sp", (XTd[:, :, 1 + ti * 128: 1 + (ti + 1) * 128], "XT_%d" % ti), xst[h2][:])
            P.barrier(bar[:])

        for l in range(n_layers):
            with ExitStack() as sm:
                def T(name, shape, dt=F32):
                    return P.sb(name, shape, dt, sm)
                mu = T("mu", [128, 14])
                omu = T("omu", [128, 14])
                gateb = T("gateb", [128, 24])
                P.dma("sp", mu[:], D["mu_fm"][l])
                P.dma("sp", gateb[:], D["gate_b_fm"][l])
                P.ts("dve", omu[:], mu[:], -1.0, ALU.mult, 1.0, ALU.add)
                hgateb = T("hgateb", [128, 24])
                P.ts("dve", hgateb[:], gateb[:], 0.5, ALU.mult)
                wbuf = [T("wbuf%d" % i, [128, 8, 256], BF16) for i in range(3)]
                wctr = [0]

                def nwb():
                    w = wbuf[wctr[0] % 3]
                    wctr[0] += 1
                    return w
                RW = P.declare(T("RW", [128, 14, TB]), 14)
                tmpA = [T("tmpA%d" % i, [128, TB]) for i in range(2)]
                QT32 = P.declare(T("QT32", [128, 4, TB]), 4)
                U32 = P.declare(T("U32", [128, 4, TB]), 4)
                Ub = P.declare(T("Ub", [128, 4, TB], BF16), 4)
                GS = P.declare(T("GS", [128, 24, TB], BF16), 24)
                WV = WINB[l].rearrange("(kc p) c -> p kc c", p=128)

                Ec = T("Ec", [128, 16, 128])
                Es = T("Es", [128, 16, 128])
                LB = T("LB", [128, 16, 2, 128], BF16)
                LC = T("LC", [128, 16, 2, 128], BF16)
                mag = T("mag", [128, 16])
                ssmd = T("ssmd", [128, 4])
                glub = T("glub", [128, 8])
                Hc = T("Hc", [128, 16, 2])
                P.dma("sp", ssmd[:], D["ssm_d_fm"][l])
                P.dma("sp", glub[:], D["glu_b_fm"][l])
                P.memset("pool", Hc[:], 0.0)
                with ExitStack() as stmp:
                    def TT(name, shape, dt=F32):
                        return P.sb(name, shape, dt, stmp)
                    wtmp = {}

                    def wrap(eng, out, x, off, shape):
                        key = (eng, tuple(shape))
                        if key not in wtmp:
                            wtmp[key] = (TT("wr_a", shape), TT("wr_i", shape, I32), TT("wr_b", shape))
                        xo, ki, kf = wtmp[key]
                        P.ts(eng, xo[:], x, float(off), ALU.add)
                        P.ts(eng, kf[:], xo[:], 1.0 / (2 * PI), ALU.mult)
                        P.cp(eng, ki[:], kf[:])
                        P.cp(eng, kf[:], ki[:])
                        P.ts(eng, kf[:], kf[:], -2 * PI, ALU.mult)
                        P.tt(eng, xo[:], xo[:], kf[:], ALU.add)
                        P.ts(eng, kf[:], xo[:], PI, ALU.is_gt, -2 * PI, ALU.mult)
                        P.tt(eng, out, xo[:], kf[:], ALU.add)

                    def s5_params(pfx, are, aim, ldt, shape):
                        t = {n: TT(pfx + n, shape) for n in ("dt", "mag", "th", "sn", "cs", "t1", "t2", "t3", "cre", "cim")}
                        P.act(t["dt"][:], ldt, AF.Exp)
                        P.tt("dve", t["t1"][:], are, t["dt"][:], ALU.mult)
                        P.act(t["mag"][:], t["t1"][:], AF.Exp)
                        P.tt("dve", t["th"][:], aim, t["dt"][:], ALU.mult)
                        wrap("dve", t["t1"][:], t["th"][:], 0.0, shape)
                        P.act(t["sn"][:], t["t1"][:], AF.Sin)
                        wrap("dve", t["t2"][:], t["th"][:], 0.5 * PI, shape)
                        P.act(t["cs"][:], t["t2"][:], AF.Sin)
                        lre, lim = t["t1"], t["t2"]
                        P.tt("dve", lre[:], t["mag"][:], t["cs"][:], ALU.mult)
                        P.tt("dve", lim[:], t["mag"][:], t["sn"][:], ALU.mult)
                        den = t["t3"]
                        P.tt("dve", den[:], are, are, ALU.mult)
                        P.tt("dve", t["cre"][:], aim, aim, ALU.mult)
                        P.tt("dve", den[:], den[:], t["cre"][:], ALU.add)
                        P.op("dve", lambda e: e.reciprocal(out=den[:], in_=den[:]), (den.name,), (den.name,))
                        P.ts("dve", lre[:], lre[:], -1.0, ALU.add)
                        P.tt("dve", t["cre"][:], lre[:], are, ALU.mult)
                        P.tt("dve", t["cim"][:], lim[:], aim, ALU.mult)
                        P.tt("dve", t["cre"][:], t["cre"][:], t["cim"][:], ALU.add)
                        P.tt("dve", t["cre"][:], t["cre"][:], den[:], ALU.mult)
                        P.tt("dve", t["cim"][:], lim[:], are, ALU.mult)
                        P.tt("dve", t["sn"][:], lre[:], aim, ALU.mult)
                        P.tt("dve", t["cim"][:], t["cim"][:], t["sn"][:], ALU.subtract)
                        P.tt("dve", t["cim"][:], t["cim"][:], den[:], ALU.mult)
                        return t

                    s5fm = TT("s5fm", [128, 3, 16])
                    P.dma("sp", s5fm[:], D["s5fm"][l])
                    pf = s5_params("f_", s5fm[:, 0, :], s5fm[:, 1, :], s5fm[:, 2, :], [128, 16])
                    P.cp("dve", mag[:], pf["mag"][:])
                    th = pf["th"]
                    angt = [TT("angt%d" % i, [128, 128]) for i in range(2)]
                    for s_ in range(16):
                        for j, (off, dst) in enumerate(((0.0, Es), (0.5 * PI, Ec))):
                            a = angt[j]
                            P.ts("pool", a[:], tvals[:], th[:, s_:s_ + 1], ALU.mult)
                            wrap("pool", a[:], a[:], off, [128, 128])
                            P.act((dst[:, s_, :], dst.name), a[:], AF.Sin)
                    s5row = TT("s5row", [128, 4, 5, 64])
                    P.dma("sp", s5row[:], D["s5row"][l])
                    pr = s5_params("r_", s5row[:, :, 0, :], s5row[:, :, 1, :], s5row[:, :, 2, :], [128, 4, 64])
                    bbr = TT("bbr", [128, 4, 64])
                    bbi = TT("bbi", [128, 4, 64])
                    tb1 = TT("tb1", [128, 4, 64])
                    P.tt("dve", bbr[:], pr["cre"][:], s5row[:, :, 3, :], ALU.mult)
                    P.tt("dve", tb1[:], pr["cim"][:], s5row[:, :, 4, :], ALU.mult)
                    P.tt("dve", bbr[:], bbr[:], tb1[:], ALU.subtract)
                    P.tt("dve", bbi[:], pr["cre"][:], s5row[:, :, 4, :], ALU.mult)
                    P.tt("dve", tb1[:], pr["cim"][:], s5row[:, :, 3, :], ALU.mult)
                    P.tt("dve", bbi[:], bbi[:], tb1[:], ALU.add)
                    for s_ in range(16):
                        kc = s_ // 4
                        for gg in range(2):
                            g8 = (2 * s_ + gg) % 8
                            for ri, src in enumerate((bbr, bbi)):
                                P.ts("pool", LB[:, s_, ri, gg * 64:(gg + 1) * 64], src[:, kc, :],
                                     rowmask[:, g8:g8 + 1], ALU.mult)
                    s5c = TT("s5c", [128, 2, 16, 16])
                    P.dma("sp", s5c[:], D["s5c"][l])
                    P.memset("pool", LC[:], 0.0)
                    for s_ in range(16):
                        for gg in range(2):
                            g8 = (2 * s_ + gg) % 8
                            P.ts("pool", LC[:, s_, 0, g8 * 16:(g8 + 1) * 16], s5c[:, 0, s_, :],
                                 halfmask[:, gg:gg + 1], ALU.mult)
                            P.ts("pool", LC[:, s_, 1, g8 * 16:(g8 + 1) * 16], s5c[:, 1, s_, :],
                                 halfmask[:, gg:gg + 1], ALU.mult, -1.0, ALU.mult)
                    P.barrier(bar[:])
                XTl = [T("s5x%d" % i, [128, 2, 2, 128]) for i in range(2)]
                S5t = [T("s5t%d" % i, [128, 2, 2, 128]) for i in range(1)]
                S5g = [T("s5g%d" % i, [128, 2, 2, 128]) for i in range(2)]
                S5h = [T("s5h%d" % i, [128, 2, 2, 128]) for i in range(1)]
                HB = [T("s5hb%d" % i, [128, 4, 2, 128], BF16) for i in range(2)]
                YG = T("YG", [128, 4, TB], BF16)
                GLb = [T("GLb%d" % i, [128, 4, 256], BF16) for i in range(2)]
                YC = T("YC", [128, 4, TB], BF16)
                SGT = T("SGT", [128, 4, TB])
                hglub = T("hglub", [128, 8])
                P.ts("dve", hglub[:], glub[:], 0.5, ALU.mult)

                def s5_block(blk):
                    for sg in range(8):
                        kc = sg // 2
                        ps = nps()
                        bu = ps[:].rearrange("p (a b t) -> p a b t", a=2, b=2)
                        for j in range(2):
                            s_ = 2 * sg + j
                            for ri in range(2):
                                P.mm(bu[:, j, ri, :], LB[:, s_, ri, :], (Ub[:, kc, :], kc))
                        xt_, tt_, g_, h_ = XTl[sg % 2], S5t[0], S5g[sg % 2], S5h[0]
                        ec = Ec[:, 2 * sg:2 * sg + 2, :]
                        es = Es[:, 2 * sg:2 * sg + 2, :]
                        P.tt("dve", xt_[:, :, 0, :], ec, bu[:, :, 0, :], ALU.mult)
                        P.tt("dve", tt_[:, :, 0, :], es, bu[:, :, 1, :], ALU.mult)
                        P.tt("dve", xt_[:, :, 1, :], ec, bu[:, :, 1, :], ALU.mult)
                        P.tt("dve", tt_[:, :, 1, :], es, bu[:, :, 0, :], ALU.mult)
                        P.tt("pool", xt_[:, :, 0, :], xt_[:, :, 0, :], tt_[:, :, 0, :], ALU.add)
                        P.tt("pool", xt_[:, :, 1, :], xt_[:, :, 1, :], tt_[:, :, 1, :], ALU.subtract)
                        for j in range(2):
                            s_ = 2 * sg + j
                            for ri in range(2):
                                P.scan(g_[:, j, ri, :], mag[:, s_:s_ + 1].to_broadcast([128, 128]), xt_[:, j, ri, :],
                                       Hc[:, s_, ri:ri + 1])
                        P.tt("pool", h_[:, :, 0, :], ec, g_[:, :, 0, :], ALU.mult)
                        P.tt("pool", tt_[:, :, 0, :], es, g_[:, :, 1, :], ALU.mult)
                        P.tt("pool", h_[:, :, 1, :], ec, g_[:, :, 1, :], ALU.mult)
                        P.tt("pool", tt_[:, :, 1, :], es, g_[:, :, 0, :], ALU.mult)
                        P.tt("pool", h_[:, :, 0, :], h_[:, :, 0, :], tt_[:, :, 0, :], ALU.subtract)
                        P.tt("pool", h_[:, :, 1, :], h_[:, :, 1, :], tt_[:, :, 1, :], ALU.add)
                        P.cp("act", Hc[:, 2 * sg:2 * sg + 2, :], h_[:, :, :, 127])
                        yield
                        hb = HB[kc % 2]
                        P.cp("act", hb[:, (sg % 2) * 2:(sg % 2) * 2 + 2, :, :], h_[:])
                        if sg % 2 == 1:
                            py = nps()
                            n = 0
                            for j in range(4):
                                s_ = kc * 4 + j
                                for ri in range(2):
                                    P.mm(py[:, 0:TB], LC[:, s_, ri, :], hb[:, j, ri, :], start=(n == 0), stop=(n == 7))
                                    n += 1
                            P.stt("dve", SGT[:, kc, :], (U32[:, kc, :], kc), ssmd[:, kc:kc + 1], py[:, 0:TB], ALU.mult, ALU.add)
                    P.act(YG[:], SGT[:], AF.Gelu)
                    yield
                    GV = GLUB[l].rearrange("(kc p) c -> p kc c", p=128)
                    for oc in range(4):
                        gl = GLb[oc % 2]
                        P.dma("sp", gl[:, :, 0:128], (GV[:, :, oc * 128:(oc + 1) * 128], "glub%d" % l))
                        P.dma("sp", gl[:, :, 128:256], (GV[:, :, (oc + 4) * 128:(oc + 5) * 128], "glub%d" % l))
                        p1 = nps()
                        p2 = nps()
                        for kc in range(4):
                            P.mm(p1[:, 0:TB], gl[:, kc, 0:128], YG[:, kc, :], start=(kc == 0), stop=(kc == 3))
                        for kc in range(4):
                            P.mm(p2[:, 0:TB], gl[:, kc, 128:256], YG[:, kc, :], start=(kc == 0), stop=(kc == 3))
                        sg_ = SGT[:, oc, :]
                        P.act(sg_, p2[:, 0:TB], AF.Tanh, bias=hglub[:, oc + 4:oc + 5], scale=0.5)
                        P.ts("pool", sg_, sg_, 0.5, ALU.mult, 0.5, ALU.add)
                        P.stt("dve", YC[:, oc, :], p1[:, 0:TB], glub[:, oc:oc + 1], sg_, ALU.add, ALU.mult)
                        yield


                QTb = P.declare(T("QTb", [128, 4, TB], BF16), 4)
                VTf = P.declare(T("VTf", [128, 4, TB], BF16), 4)
                KTb = P.declare(T("KTb", [128, 4, 256], BF16), 4)
                Vb = T("Vb", [128, 2, 8, 65], BF16)
                P.memset("pool", Vb[:], 1.0)
                ksum = T("ksum", [128, 4, 32])
                kmean = T("kmean", [128, 4, 16])
                KTs = [T("KTs%d" % i, [128, 4, 256], BF16) for i in range(2)]
                Vs = [T("Vs%d" % i, [128, 2, 8, 65], BF16) for i in range(2)]
                Gt = T("Gt", [128, 8, 16])
                M8 = T("M8", [128, 8, 8])
                CQ = T("CQ", [128, 8, 16])
                slq = T("slq", [128, 8])
                Pb = [T("Pb%d" % i, [128, 2, 256], BF16) for i in range(4)]
                PT = [T("PT%d" % i, [128, 4, 128], BF16) for i in range(4)]
                RS = T("RS", [128, 8, 17])
                lsum = T("lsum", [128, 8])
                Ofin = T("Ofin", [128, 8, 64], BF16)
                YB = T("YB", [128, 4, TB], BF16)
                mctr = [0]

                def moba_block(blk):
                    mb, half = blk // 2, blk % 2
                    q0 = blk * 128
                    nk = 128 * (half + 1)
                    pb = PSB[blk % 2]
                    for hp in range(4):
                        P.tr(pb[:, hp * 128:(hp + 1) * 128], VTf[:, hp, :], identb[:])
                    P.cp("dve", Vb[:, half, :, 0:64], pb[:, 0:512].rearrange("p (h d) -> p h d", h=8))
                    P.memset("pool", RS[:], 0.0)
                    P.ts("pool", slq[:], mbSl[:], float(-q0), ALU.mult)
                    if mb > 0:
                        pgs = (nps(), nps())
                        P.memset("pool", Gt[:], -1e30)
                        for h in range(8):
                            hp, e = h // 2, h % 2
                            P.mm(pgs[e][:, hp * 16:hp * 16 + mb], (QT32[e * 64:(e + 1) * 64, hp, :], hp),
                                 kmean[e * 64:(e + 1) * 64, hp, 0:mb])
                        for e in range(2):
                            P.cp("dve", Gt[:, e:8:2, 0:mb], pgs[e][:, 0:64].rearrange("p (a n) -> p a n", a=4)[:, :, 0:mb])
                        for h in range(8):
                            P.op("dve", lambda e_, h=h: e_.max(out=M8[:, h, :], in_=Gt[:, h, :]), (Gt.name,), (M8.name,))
                        for h in range(8):
                            P.ts("dve", CQ[:, h, :], Gt[:, h, :], M8[:, h, 2:3], ALU.is_lt, -30000.0, ALU.mult)
                        P.memset("dve", CQ[:, :, mb:mb + 1], 0.0)
                        P.tt("dve", CQ[:], CQ[:], mbT1[:], ALU.add)
                    else:
                        P.cp("dve", CQ[:], mbT1[:])
                    P.tt("dve", CQ[:], CQ[:], slq[:].unsqueeze(2).to_broadcast([128, 8, 16]), ALU.add)
                    yield
                    po = PSF[4]
                    pov = po[:].rearrange("p (h d) -> p h d", h=8)
                    first = [True]
                    nlast = [0]
                    loaded = {}

                    def kv(n):
                        if n == mb:
                            return KTb, Vb, nk, True
                        if n not in loaded:
                            kt_, v_ = KTs[n % 2], Vs[n % 2]
                            P.dma("sp", kt_[:], (KTd[n], "ktd%d" % n))
                            P.dma("sp", v_[:].rearrange("p a h d -> p a (h d)"), (Vd[n], "vd%d" % n))
                            loaded[n] = (kt_, v_)
                        kt_, v_ = loaded[n]
                        return kt_, v_, 256, False

                    def heads(hps):
                        for n in range(mb + 1):
                            kt_, v_, ncols, own = kv(n)
                            for hp in hps:
                                pse = (nps(), nps())
                                pbuf = Pb[hp]
                                for e in range(2):
                                    h = 2 * hp + e
                                    pv_ = pse[e]
                                    P.mm(pv_[:, 0:ncols], (QTb[e * 64:(e + 1) * 64, hp, :], hp),
                                         ((kt_[e * 64:(e + 1) * 64, hp, 0:ncols], hp) if own else kt_[e * 64:(e + 1) * 64, hp, 0:ncols]), start=True, stop=False)
                                    P.mm(pv_[:, 0:ncols], ones2[e * 64:e * 64 + 2, :], ALR[e * 64:e * 64 + 2, h, 0:ncols], start=False, stop=(not own))
                                    if own:
                                        c0 = half * 128
                                        P.mm(pv_[:, c0:c0 + 128], identb[:], trib[:], start=False, stop=True)
                                for e in range(2):
                                    h = 2 * hp + e
                                    P.act(pbuf[:, e, 0:ncols], pse[e][:, 0:ncols], AF.Exp, bias=CQ[:, h, n:n + 1],
                                          accum=RS[:, h, n:n + 1])
                                yield
                                ptb = PSB[hp % 2]
                                nt = ncols // 128
                                for e in range(2):
                                    for kt in range(nt):
                                        P.tr(ptb[:, (e * 2 + kt) * 128:(e * 2 + kt + 1) * 128], pbuf[:, e, kt * 128:(kt + 1) * 128], identb[:])
                                ptile = PT[hp]
                                if nt == 2:
                                    P.cp("dve", ptile[:], ptb[:, 0:512].rearrange("p (a q) -> p a q", a=4))
                                else:
                                    P.cp("dve", ptile[:, 0:1, :], ptb[:, 0:128].rearrange("p (a q) -> p a q", a=1))
                                    P.cp("dve", ptile[:, 2:3, :], ptb[:, 256:384].rearrange("p (a q) -> p a q", a=1))
                                for e in range(2):
                                    h = 2 * hp + e
                                    for kt in range(nt):
                                        nlast[0] += 1
                                        P.mm(pov[:, h, :], ptile[:, e * 2 + kt, :], (v_[:, kt, h, 0:64], v_.name),
                                             start=first[0], stop=False, skip_group_check=True)
                                        first[0] = False
                                yield
                    subs = [heads((0, 1)), heads((2, 3))]
                    while subs:
                        for g_ in list(subs):
                            try:
                                next(g_)
                            except StopIteration:
                                subs.remove(g_)
                        yield
                    P.op("dve", lambda e_: e_.reduce_sum(out=lsum[:], in_=RS[:], axis=AX.X), (RS.name,), (lsum.name,))
                    P.op("dve", lambda e_: e_.reciprocal(out=lsum[:], in_=lsum[:]), (lsum.name,), (lsum.name,))
                    P.tt("dve", Ofin[:], pov, lsum[:].unsqueeze(2).to_broadcast([128, 8, 64]), ALU.mult)
                    pb2 = PSB[blk % 2]
                    of = Ofin[:].rearrange("p h d -> p (h d)")
                    for c in range(4):
                        P.tr(pb2[:, c * 128:(c + 1) * 128], of[:, c * 128:(c + 1) * 128], identb[:])
                    P.cp("act", YB[:], pb2[:, 0:512].rearrange("p (c t) -> p c t", c=4))
                    if half == 1:
                        P.dma("pool", (KTd[mb], "ktd%d" % mb), KTb[:])
                        P.dma("pool", (Vd[mb], "vd%d" % mb), Vb[:].rearrange("p a h d -> p a (h d)"))
                        P.tt("pool", kmean[:, :, mb], ksum[:, :, 2 * mb], ksum[:, :, 2 * mb + 1], ALU.add)
                        P.ts("pool", kmean[:, :, mb], kmean[:, :, mb], 1.0 / 256.0, ALU.mult)


                CDEC = math.exp(-0.5)
                rwv = T("rwv", [128, 5, 4])
                P.dma("sp", rwv[:], D["rw_vecs"][l])
                hrw = T("hrw", [128, 2, 4])
                P.ts("dve", hrw[:], rwv[:, 0:2, :], 0.5, ALU.mult)
                w2a2f = T("w2a2f", [128, 512])
                W2A2 = T("W2A2", [128, 512], BF16)
                P.dma("sp", w2a2f[:], D["rw_w2a2"][l])
                P.cp("dve", W2A2[:], w2a2f[:])
                G2 = T("G2", [128, 512], BF16)
                P.dma("sp", w2a2f[:], D["rw_g2"][l])
                P.cp("dve", G2[:], w2a2f[:])
                lnwb = T("lnwb", [64, 2, 512])
                P.dma("sp", lnwb[:], D["rw_ln"][l])
                ST = T("ST", [64, 8, 64])
                P.memset("pool", ST[:], 0.0)
                LW = T("LW", [128, TB], BF16)
                SGx = T("SGx", [128, TB], BF16)
                F1 = [T("rwf%d" % i, [128, 4, TB]) for i in range(6)]
                F1x = T("rwfx", [128, 4, TB])
                Bq = {n: T("rwb_" + n, [128, 4, TB], BF16) for n in ("At", "Rt", "Bt", "Kt", "Bh", "Kh", "Vf", "Rk")}
                WCf = T("WCf", [128, 4, 2])
                WCt = T("WCt", [64, 4, 2, 2])
                TMc = T("TMc", [64, 5, 4, 128], BF16)
                SC = {n: T("rws_" + n, [64, 8, 64], BF16) for n in ("P", "PT", "Pn", "PnT", "ArbT", "LakT", "ArkT")}
                Zb = [T("rwZ%d" % i, [64, 8, 128], BF16) for i in range(2)]
                MT = T("rwMT", [64, 8, 64])
                DW = T("rwDW", [64, 8, 64])
                RpT = T("rwRpT", [64, 8, 64])
                Y32 = [T("rwY%d" % i, [64, 8, 64]) for i in range(2)]
                st1 = T("rwst1", [64, 8])
                st2 = T("rwst2", [64, 8])
                bsc = T("rwbs", [64, 8])
                YAt = T("YAt", [64, 512], BF16)
                YA = T("YA", [128, 4, TB], BF16)

                def bc4(col):
                    return col.unsqueeze(2).to_broadcast([128, 4, TB])

                def rwkv_block(blk):
                    r_, k_, v_ = RW[:, 0:4, :], RW[:, 4:8, :], RW[:, 8:12, :]
                    f0, f1, f2, f3, f4, f5 = F1
                    yield
                    P.act(LW[0:64, :], (RW[0:64, 12, :], 12), AF.Tanh)
                    P.cp("pool", LW[64:128, :], (RW[64:128, 12, :], 12))
                    P.act(SGx[:], (RW[:, 13, :], 13), AF.Tanh, scale=0.5)
                    P.ts("pool", SGx[:], SGx[:], 0.5, ALU.mult, 0.5, ALU.add)
                    pw = nps()
                    pa = nps()
                    pwv = pw[:].rearrange("p (a t) -> p a t", a=4)
                    pav = pa[:].rearrange("p (a t) -> p a t", a=4)
                    for hp in range(4):
                        P.mm(pwv[:, hp, :], W2A2[0:64, hp * 128:(hp + 1) * 128], LW[0:64, :])
                        P.mm(pav[:, hp, :], W2A2[64:128, hp * 128:(hp + 1) * 128], LW[64:128, :])
                    sgw, ai = f0, f1
                    for hp in range(4):
                        P.act(sgw[:, hp, :], pwv[:, hp, :], AF.Tanh, bias=hrw[:, 0, hp:hp + 1], scale=0.5)
                        P.act(ai[:, hp, :], pav[:, hp, :], AF.Tanh, bias=hrw[:, 1, hp:hp + 1], scale=0.5)
                    P.ts("pool", sgw[:], sgw[:], 0.5, ALU.mult, 0.5, ALU.add)
                    P.ts("pool", ai[:], ai[:], 0.5, ALU.mult, 0.5, ALU.add)
                    yield
                    kkr = f2
                    P.tt("pool", kkr[:], k_, bc4(rwv[:, 2, :]), ALU.mult)
                    sqb = Bq["Rk"]
                    P.tt("pool", sqb[:], kkr[:], kkr[:], ALU.mult)
                    pss = nps()
                    P.mm(pss[:], BOb[:], sqb[:].rearrange("p a t -> p (a t)"))
                    rn = f3
                    rnf = rn[:].rearrange("p a t -> p (a t)")
                    P.act(rnf, pss[:], AF.Sqrt)
                    P.ts("dve", rnf, rnf, 1e-12, ALU.max)
                    P.op("dve", lambda e_: e_.reciprocal(out=rnf, in_=rnf), (rn.name,), (rn.name,))
                    P.tt("pool", kkr[:], kkr[:], rn[:], ALU.mult)
                    yield
                    km = f3
                    P.stt("dve", km[:], ai[:], -1.0, bc4(rwv[:, 3, :]), ALU.add, ALU.mult)
                    P.ts("dve", km[:], km[:], 1.0, ALU.add)
                    P.tt("dve", km[:], km[:], k_, ALU.mult)
                    yield
                    P.tt("pool", f4[:], r_, bc4(rwv[:, 4, :]), ALU.mult)
                    P.tt("pool", Bq["Rk"][:], f4[:], km[:], ALU.mult)
                    yield
                    cs = f4
                    for hp in range(4):
                        for c in range(2):
                            P.scan(cs[:, hp, c * 64:(c + 1) * 64], ones64[:, 0:64], sgw[:, hp, c * 64:(c + 1) * 64], 0.0)
                    bkk = f5
                    P.tt("pool", bkk[:], kkr[:], ai[:], ALU.mult)
                    yield
                    ep, em = f1, f0
                    dprev = F1x
                    P.tt("dve", dprev[:], cs[:], sgw[:], ALU.subtract)
                    P.act(dprev[:], dprev[:], AF.Exp, scale=-CDEC)
                    P.stt("dve", Bq["At"][:], kkr[:], -1.0, dprev[:], ALU.mult, ALU.mult)
                    P.act(ep[:], cs[:], AF.Exp, scale=-CDEC)
                    P.tt("pool", Bq["Rt"][:], r_, ep[:], ALU.mult)
                    P.act(em[:], cs[:], AF.Exp, scale=CDEC)
                    P.tt("dve", Bq["Bt"][:], bkk[:], em[:], ALU.mult)
                    P.tt("pool", Bq["Kt"][:], km[:], em[:], ALU.mult)
                    yield
                    csv = cs[:].rearrange("p a (c t) -> p a c t", c=2)
                    dv = dprev[:].rearrange("p a (c t) -> p a c t", c=2)
                    P.tt("dve", dv, csv, csv[:, :, :, 63:64].to_broadcast([128, 4, 2, 64]), ALU.subtract)
                    P.act(dprev[:], dprev[:], AF.Exp, scale=CDEC)
                    P.tt("dve", Bq["Bh"][:], bkk[:], dprev[:], ALU.mult)
                    P.tt("pool", Bq["Kh"][:], km[:], dprev[:], ALU.mult)
                    P.act(WCf[:], csv[:, :, :, 63], AF.Exp, scale=-CDEC)
                    P.cp("pool", Bq["Vf"][:], v_)
                    yield
                    pwc = nps()
                    P.mm(pwc[0:64, 0:8], identf[:, 64:128], WCf[:].rearrange("p a c -> p (a c)"))
                    P.cp("dve", WCt[:, :, 0, :], WCf[0:64, :, :])
                    P.cp("dve", WCt[:, :, 1, :], pwc[0:64, 0:8].rearrange("p (a c) -> p a c", a=4))
                    for c in range(2):
                        if RWCUT > 0:
                            yield from rwkv_chunk(blk, c)

                def rwkv_chunk(blk, c):
                    tc = slice(c * 64, (c + 1) * 64)
                    yield
                    srcs = [Bq["At"], Bq["Bh"], Bq["Kh"], Bq["Vf"], Bq["Rt"]]
                    n = 0
                    for gi_, grp in enumerate(((0, 1), (2, 3), (4,))):
                        pb = PSB[(c + gi_) % 2]
                        for a_, si in enumerate(grp):
                            for hp in range(4):
                                col = (a_ * 4 + hp) * 128
                                P.tr(pb[0:64, col:col + 128], srcs[si][:, hp, tc], identb[:])
                        na = len(grp)
                        P.cp("act" if gi_ % 2 else "dve", TMc[:, grp[0]:grp[0] + na, :, :],
                             pb[0:64, 0:na * 512].rearrange("p (a h x) -> p a h x", a=na, h=4))

                    def tm(i, h):
                        return TMc[:, i, h // 2, (h % 2) * 64:(h % 2) * 64 + 64]

                    def fmr(name, h):
                        return Bq[name][(h % 2) * 64:(h % 2) * 64 + 64, h // 2, tc]
                    if RWCUT < 2:
                        return
                    yield
                    def score(dst, lname, rname, mi):
                        banks = (nps(), nps())
                        for h in range(8):
                            P.mm(banks[h % 2][0:64, (h // 2) * 64:(h // 2) * 64 + 64], fmr(lname, h), fmr(rname, h))
                        for e in range(2):
                            P.tt("dve", SC[dst][:, e:8:2, :], banks[e][0:64, 0:256].rearrange("p (a s) -> p a s", a=4),
                                 rwmask[:, mi:mi + 1, :].to_broadcast([64, 4, 64]), ALU.mult)
                    score("P", "At", "Bt", 0)
                    score("PT", "Bt", "At", 1)
                    score("LakT", "Kt", "At", 1)
                    score("ArbT", "Bt", "Rt", 2)
                    score("ArkT", "Kt", "Rt", 2)
                    if RWCUT < 3:
                        return
                    yield
                    pz = nps()
                    pzv = pz[0:64, :].rearrange("p (h s) -> p h s", h=8)
                    for h in range(8):
                        P.mm(pzv[:, h, :], SC["LakT"][:, h, :], tm(3, h))
                    Z = Zb[0]
                    P.cp("act", Z[:, :, 64:128], pzv)
                    P.cp("pool", Z[:, :, 0:64], TMc[:, 0, :, :].rearrange("p a (e x) -> p (a e) x", e=2))
                    if RWCUT < 4:
                        return
                    yield
                    Pk, PkT, Pn, PnT = SC["P"], SC["PT"], SC["Pn"], SC["PnT"]
                    for k in range(6):
                        Zn = Zb[(k + 1) % 2]
                        for hh in range(2):
                            pzz = nps()
                            pzzv = pzz[0:64, :].rearrange("p (h s) -> p h s", h=4)
                            for h4 in range(4):
                                h = hh * 4 + h4
                                P.mm(pzzv[:, h4, :], PkT[:, h, :], Z[:, h, :], start=(h4 == 0), stop=True, skip_group_check=True)
                            P.tt("dve", Zn[:, hh * 4:(hh + 1) * 4, :], pzzv, Z[:, hh * 4:(hh + 1) * 4, :], ALU.add)
                        Z = Zn
                        if k < 5:
                            ppT = nps()
                            ppTv = ppT[0:64, :].rearrange("p (h s) -> p h s", h=8)
                            for h in range(8):
                                P.mm(ppTv[:, h, :], Pk[:, h, :], PkT[:, h, :])
                            P.cp("dve", PnT[:], ppTv)
                        if k < 4:
                            pp = nps()
                            ppv = pp[0:64, :].rearrange("p (h s) -> p h s", h=8)
                            for h in range(8):
                                P.mm(ppv[:, h, :], PkT[:, h, :], Pk[:, h, :])
                            P.cp("act", Pn[:], ppv)
                        Pk, PkT, Pn, PnT = Pn, PnT, Pk, PkT
                        yield
                    if RWCUT < 5:
                        return
                    yield
                    yield
                    pM = nps()
                    pMv = pM[0:64, :].rearrange("p (h s) -> p h s", h=8)
                    for h in range(8):
                        P.mm(pMv[:, h, :], Z[:, h, 0:64], tm(1, h))
                    wcv = WCt[:, :, :, c].rearrange("p a e -> p (a e)")
                    P.tt("pool", DW[:], identf[0:64, 0:64].unsqueeze(1).to_broadcast([64, 8, 64]),
                         wcv.unsqueeze(2).to_broadcast([64, 8, 64]), ALU.mult)
                    P.tt("dve", MT[:], pMv, DW[:], ALU.add)
                    yield
                    pR = nps()
                    pRv = pR[0:64, :].rearrange("p (h s) -> p h s", h=8)
                    for h in range(8):
                        P.mm(pRv[:, h, :], Z[:, h, 0:64], SC["ArbT"][:, h, :], start=(h == 0), stop=False, skip_group_check=True)
                        P.mm(pRv[:, h, :], tm(4, h), identb[0:64, 0:64], start=False, stop=True, skip_group_check=True)
                    P.cp("act", RpT[:], pRv)
                    if RWCUT < 6:
                        return
                    yield
                    pY = nps()
                    pYv = pY[0:64, :].rearrange("p (h s) -> p h s", h=8)
                    for h in range(8):
                        P.mm(pYv[:, h, :], RpT[:, h, :], ST[:, h, :], start=(h == 0), stop=False, skip_group_check=True)
                        P.mm(pYv[:, h, :], SC["ArbT"][:, h, :], Z[:, h, 64:128], start=False, stop=False, skip_group_check=True)
                        P.mm(pYv[:, h, :], SC["ArkT"][:, h, :], tm(3, h), start=False, stop=True, skip_group_check=True)
                    y = Y32[c % 2]
                    P.cp("act", y[:], pYv)
                    yield
                    pS = PSF[5]
                    pSv = pS[0:64, :].rearrange("p (h s) -> p h s", h=8)
                    for h in range(8):
                        P.mm(pSv[:, h, :], MT[:, h, :], ST[:, h, :], start=(h == 0), stop=False, skip_group_check=True)
                        P.mm(pSv[:, h, :], tm(1, h), Z[:, h, 64:128], start=False, stop=False, skip_group_check=True)
                        P.mm(pSv[:, h, :], tm(2, h), tm(3, h), start=False, stop=True, skip_group_check=True)
                    P.cp("dve", ST[:], pSv)
                    if RWCUT < 7:
                        return
                    yield
                    sq = Y32[(c + 1) % 2]
                    P.op("dve", lambda e_: e_.reduce_sum(out=st1[:], in_=y[:], axis=AX.X), (y.name,), (st1.name,))
                    P.tt("pool", sq[:], y[:], y[:], ALU.mult)
                    P.op("dve", lambda e_: e_.reduce_sum(out=st2[:], in_=sq[:], axis=AX.X), (sq.name,), (st2.name,))
                    P.ts("dve", st1[:], st1[:], 1.0 / 64, ALU.mult)
                    P.ts("dve", st2[:], st2[:], 1.0 / 64, ALU.mult)
                    P.tt("dve", bsc[:], st1[:], st1[:], ALU.mult)
                    P.tt("dve", st2[:], st2[:], bsc[:], ALU.subtract)
                    P.ts("dve", st2[:], st2[:], 64e-5, ALU.add)
                    P.act(st2[:], st2[:], AF.Sqrt)
                    P.op("dve", lambda e_: e_.reciprocal(out=st2[:], in_=st2[:]), (st2.name,), (st2.name,))
                    P.tt("pool", y[:], y[:], st1[:].unsqueeze(2).to_broadcast([64, 8, 64]), ALU.subtract)
                    P.tt("pool", y[:], y[:], st2[:].unsqueeze(2).to_broadcast([64, 8, 64]), ALU.mult)
                    yf = y[:].rearrange("p h i -> p (h i)")
                    P.tt("pool", yf, yf, lnwb[:, 0, :], ALU.mult)
                    P.tt("pool", yf, yf, lnwb[:, 1, :], ALU.add)
                    yield
                    pbs = nps()
                    for hp in range(4):
                        P.mm(pbs[0:64, hp * 2:hp * 2 + 2], Bq["Rk"][:, hp, tc], BOb[:, 0:128:64])
                    P.cp("dve", bsc[:], pbs[0:64, 0:8])
                    vtm = TMc[:, 3, :, :].rearrange("p a (e x) -> p (a e) x", e=2)
                    P.tt("pool", sq[:], vtm, bsc[:].unsqueeze(2).to_broadcast([64, 8, 64]), ALU.mult)
                    P.tt("pool", y[:], y[:], sq[:], ALU.add)
                    yield
                    pG = nps()
                    P.mm(pG[0:64, :], SGx[:, tc], G2[:])
                    P.tt("dve", YAt[:], yf, pG[0:64, :], ALU.mult)
                    pb = PSB[c % 2]
                    for cc in range(4):
                        P.tr(pb[:, cc * 64:(cc + 1) * 64], YAt[:, cc * 128:(cc + 1) * 128], identb[0:64, 0:64])
                    P.cp("act", YA[:, :, tc], pb[:, 0:256].rearrange("p (a t) -> p a t", a=4))


                lnw1 = T("lnw1", [128, 1024])
                lnb1 = T("lnb1", [128, 1024])
                P.dma("sp", lnw1[:], D["ln1_rep"][l, 0])
                P.dma("sp", lnb1[:], D["ln1_rep"][l, 1])
                Hs = T("Hs", [128, 1024])
                xr = T("xr", [128, 1024])
                MG = T("MG", [128, 8, TB], BF16)
                lst = T("lst", [128, 4])
                XRES = D["x"] if l == 0 else X2

                def merge_block(blk):
                    t0 = blk * TB
                    P.dma("sp", xr[:], (XRES[t0:t0 + TB, :], "xres_%d" % blk))
                    hsv = Hs[:].rearrange("p (a t) -> p a t", a=8)
                    for half in range(2):
                        pus = []
                        for br, Y in enumerate((YA, YB, YC)):
                            wb = nwb()
                            wv = wb[:].rearrange("p a b -> p (a b)").rearrange("p (kc c) -> p kc c", kc=4)
                            P.dma("sp", (wv, wb.name), (UPB[l, br].rearrange("(kc p) c -> p kc c", p=128)[:, :, half * 512:(half + 1) * 512], "upb%d" % l))
                            pu = nps()
                            puv = pu[:].rearrange("p (a t) -> p a t", a=4)
                            for d4 in range(4):
                                for kc in range(4):
                                    P.mm(puv[:, d4, :], (wv[:, kc, d4 * 128:(d4 + 1) * 128], wb.name), Y[:, kc, :],
                                         start=(kc == 0 and d4 == 0), stop=(kc == 3), skip_group_check=True)
                            pus.append(puv)
                        a0_, a1_ = hsv[:, 0:4, :], hsv[:, 4:8, :]
                        g0 = half * 4
                        P.stt("dve", a0_, GS[:, g0:g0 + 4, :], 1.0, pus[0], ALU.add, ALU.mult)
                        P.stt("dve", a1_, GS[:, 8 + g0:8 + g0 + 4, :], 1.0, pus[1], ALU.add, ALU.mult)
                        P.tt("pool", a0_, a0_, a1_, ALU.add)
                        P.stt("dve", a1_, GS[:, 16 + g0:16 + g0 + 4, :], 1.0, pus[2], ALU.add, ALU.mult)
                        P.tt("pool", MG[:, g0:g0 + 4, :], a0_, a1_, ALU.add)
                    for cg in range(4):
                        wb = nwb()
                        P.dma("sp", wb[:], (WOB[l].rearrange("(kc p) c -> p kc c", p=128)[:, :, cg * 256:(cg + 1) * 256], "wob%d" % l))
                        ph = nps()
                        for kc in range(8):
                            P.mm(ph[:, 0:256], MG[:, kc, :], wb[:, kc, :], start=(kc == 0), stop=(kc == 7))
                        P.stt("dve", Hs[:, cg * 256:(cg + 1) * 256], xr[:, cg * 256:(cg + 1) * 256], float(2 * ALPHA), ph[:, 0:256], ALU.mult, ALU.add)
                    layer_norm(Hs, xr, lst, lnw1, lnb1, eps=4e-5)
                    P.dma("pool", (X1[t0:t0 + TB, :], "x1_%d" % blk), Hs[:])

                def layer_norm(h_, junk, st_, w_, b_, eps=1e-5):
                    P.act(junk[:], h_[:], AF.Copy, accum=st_[:, 0:1])
                    P.act(junk[:], h_[:], AF.Square, accum=st_[:, 1:2])
                    P.ts("dve", st_[:, 0:2], st_[:, 0:2], 1.0 / DM, ALU.mult)
                    P.tt("dve", st_[:, 2:3], st_[:, 0:1], st_[:, 0:1], ALU.mult)
                    P.tt("dve", st_[:, 1:2], st_[:, 1:2], st_[:, 2:3], ALU.subtract)
                    P.ts("dve", st_[:, 1:2], st_[:, 1:2], float(eps), ALU.add)
                    P.act(st_[:, 1:2], st_[:, 1:2], AF.Sqrt)
                    P.op("dve", lambda e_: e_.reciprocal(out=st_[:, 1:2], in_=st_[:, 1:2]), (st_.name,), (st_.name,))
                    P.ts("dve", h_[:], h_[:], st_[:, 0:1], ALU.subtract, st_[:, 1:2], ALU.mult)
                    P.tt("dve", h_[:], h_[:], w_[:], ALU.mult)
                    P.tt("dve", h_[:], h_[:], b_[:], ALU.add)

                ngrp = 27
                xtb = [T("xtb%d" % i, [128, 8, TB + 1], BF16) for i in range(2)]
                def inproj(blk, xt, g0, g1):
                    for g in range(g0, g1):
                        wb = nwb()
                        ncol = 256
                        P.dma("sp", (wb[:, :, 0:ncol], wb.name), (WV[:, :, g * 256: g * 256 + ncol], "winb%d" % l))
                        for mm_ in range(ncol // 128):
                            m = g * 2 + mm_
                            ps = nps()
                            shift = m < 14
                            n = TB + 1 if shift else TB
                            c0 = 0 if shift else 1
                            for kc in range(8):
                                P.mm(ps[:, 0:n], (wb[:, kc, mm_ * 128:(mm_ + 1) * 128], wb.name),
                                     xt[:, kc, c0:c0 + n], start=(kc == 0), stop=(kc == 7))
                            if shift:
                                ta = tmpA[m % 2]
                                P.act(ta[:], ps[:, 1:TB + 1], AF.Copy, scale=omu[:, m:m + 1])
                                P.stt("dve", (RW[:, m, :], m), ps[:, 0:TB], mu[:, m:m + 1], ta[:], ALU.mult, ALU.add)
                            elif m < 18:
                                P.act((QT32[:, m - 14, :], m - 14), ps[:, 0:TB], AF.Copy, scale=0.125)
                                P.cp("pool", (QTb[:, m - 14, :], m - 14), (QT32[:, m - 14, :], m - 14))
                            elif m < 22:
                                hf = blk % 2
                                P.act((KTb[:, m - 18, hf * 128:(hf + 1) * 128], m - 18), ps[:, 0:TB], AF.Copy,
                                      accum=ksum[:, m - 18, blk:blk + 1])
                            elif m < 26:
                                P.act((VTf[:, m - 22, :], m - 22), ps[:, 0:TB], AF.Copy)
                            elif m < 30:
                                P.act((U32[:, m - 26, :], m - 26), ps[:, 0:TB], AF.Copy)
                                P.cp("pool", (Ub[:, m - 26, :], m - 26), (U32[:, m - 26, :], m - 26))
                            else:
                                P.act((GS[:, m - 30, :], m - 30), ps[:, 0:TB], AF.Tanh, bias=hgateb[:, m - 30:m - 29], scale=0.5)
                        yield

                def load_xt(blk):
                    xt_ = xtb[blk % 2]
                    tt0 = blk * TB
                    P.dma_fn("sp", lambda e, xt_=xt_, tt0=tt0: e.dma_start(out=xt_[:], in_=XTd[:, :, tt0:tt0 + TB + 1]),
                             ("XT_%d" % blk, ("XT_%d" % (blk - 1)) if blk else "XTz"), (xt_.name,))

                load_xt(0)
                for blk in range(nblk):
                    t0 = blk * TB
                    xt = xtb[blk % 2]
                    for _ in inproj(blk, xt, 0, 15):
                        pass
                    if blk + 1 < nblk:
                        load_xt(blk + 1)
                    gens = [inproj(blk, xt, 15, 27)]
                    if 'rwkv' not in SKIP:
                        gens.append(rwkv_block(blk))
                    if 'moba' not in SKIP:
                        gens.append(moba_block(blk))
                    if 's5blk' not in SKIP and 's5' not in SKIP:
                        gens.append(s5_block(blk))
                    gate_gen = gens[0]
                    rnd = 0
                    while gens:
                        for g_ in list(gens):
                            reps = 3 if (g_ is gate_gen and rnd < 4) else 1
                            for _r in range(reps):
                                try:
                                    next(g_)
                                except StopIteration:
                                    if g_ in gens:
                                        gens.remove(g_)
                                    break
                        rnd += 1
                    if 'merge' not in SKIP:
                        merge_block(blk)
                    if debug and blk == dbg_blk and l == 0:
                        d32 = T("d32", [128, 4, TB])
                        if "X1" in dbg and "merge" not in SKIP:
                            P.dma("sp", dbg["X1"], Hs[:])
                        if "YA" in dbg and "rwkv" not in SKIP and RWCUT >= 7:
                            P.cp("dve", d32[:, 0:4, :], YA[:])
                            P.dma("sp", dbg["YA"], d32[:, 0:4, :])
                        if "YB" in dbg and "moba" not in SKIP:
                            P.cp("dve", d32[:, 0:4, :], YB[:])
                            P.dma("sp", dbg["YB"], d32[:, 0:4, :])
                        if "YC" in dbg:
                            P.cp("dve", d32[:, 0:4, :], YC[:])
                            P.dma("sp", dbg["YC"], d32[:, 0:4, :])
                print('sbuf remaining (mixer phase)', nc.sbuf_bytes_remaining)
                P.barrier(bar[:])

            if 'moe' in SKIP:
                continue
            with ExitStack() as se:
                def T(name, shape, dt=F32):
                    return P.sb(name, shape, dt, se)
                last = (l == n_layers - 1)
                NT = nblk
                NB = 2 * NT + 32
                rwt = T("m_rw", [128, 8, 36])
                rbt = T("m_rb", [128, 36])
                trib_s = T("m_tri", [128, 128], BF16)
                onesb = T("m_ones", [128, 128], BF16)
                thr = T("m_thr", [128, 64])
                nv = T("m_nv", [128, 96])
                pvt = T("m_pv", [128, 1])
                lnw2 = T("lnw2", [128, 1024])
                lnb2 = T("lnb2", [128, 1024])
                tmp32 = T("m_tmp32", [128, 128])
                P.dma("sp", rwt[:], D["moe_rw"][l])
                P.dma("sp", rbt[:], D["moe_rb"][l])
                P.dma("sp", tmp32[:], D["moe_tri"][:, :])
                P.cp("dve", trib_s[:], tmp32[:])
                P.memset("pool", onesb[:], 1.0)
                P.dma("sp", thr[:], D["moe_thr"][:, :])
                P.dma("sp", nv[:], D["moe_nv"][:, :])
                P.dma("sp", pvt[:], D["moe_pv"][:, :])
                P.dma("sp", lnw2[:], D["ln2_rep"][l, 0])
                P.dma("sp", lnb2[:], D["ln2_rep"][l, 1])
                OHK = T("m_ohk", [128, NT, 2, 32])
                OHS = T("m_ohs", [128, NT, 32], BF16)
                PF = T("m_pf", [128, NT, 32])
                WT = T("m_wt", [128, NT, 2])
                cnt = T("m_cnt", [128, 32])
                P.memset("pool", cnt[:], 0.0)
                xt32 = [T("m_x%d" % i, [128, 1024]) for i in range(2)]
                xT32 = T("m_xT", [128, 8, 128])
                LG = T("m_lg", [128, 36])
                sm_ = {n: T("m_s_" + n, [128, 8]) for n in ("a", "b", "c", "d", "m8")}
                g4 = {n: T("m_g_" + n, [128, 4]) for n in ("oh", "e", "t")}
                e32 = T("m_e32", [128, 4, 8])
                for ti in range(NT):
                    x_ = xt32[ti % 2]
                    P.dma("sp", x_[:], (X1[ti * 128:(ti + 1) * 128, :], "x1_%d" % ti))
                    for hh in range(2):
                        pt = nps()
                        for k4 in range(4):
                            kc = hh * 4 + k4
                            P.tr(pt[:, k4 * 128:(k4 + 1) * 128], x_[:, kc * 128:(kc + 1) * 128], identf[:])
                        P.cp("act" if hh else "dve", xT32[:, hh * 4:(hh + 1) * 4, :], pt[:].rearrange("p (a t) -> p a t", a=4))
                    pl = nps()
                    for kc in range(8):
                        P.mm(pl[:, 0:36], xT32[:, kc, :], rwt[:, kc, :], start=(kc == 0), stop=(kc == 7))
                    P.tt("dve", LG[:], pl[:, 0:36], rbt[:], ALU.add)
                    a_, b_, c_, d_, m8 = sm_["a"], sm_["b"], sm_["c"], sm_["d"], sm_["m8"]
                    P.op("dve", lambda e_: e_.reduce_max(out=a_[:, 0:1], in_=LG[:, 0:4], axis=AX.X), (LG.name,), (a_.name,))
                    P.ts("dve", g4["oh"][:], LG[:, 0:4], a_[:, 0:1], ALU.is_equal)
                    P.ts("dve", a_[:, 1:2], a_[:, 0:1], -1.0, ALU.mult)
                    P.act(g4["e"][:], LG[:, 0:4], AF.Exp, bias=a_[:, 1:2], accum=a_[:, 2:3])
                    P.op("dve", lambda e_: e_.reciprocal(out=a_[:, 3:4], in_=a_[:, 2:3]), (a_.name,), (a_.name,))
                    P.tt("dve", e32[:], LG[:, 4:36].rearrange("p (g e) -> p g e", g=4),
                         g4["oh"][:].unsqueeze(2).to_broadcast([128, 4, 8]), ALU.mult)
                    P.op("dve", lambda e_: e_.reduce_sum(out=b_[:], in_=e32[:].rearrange("p g e -> p e g"), axis=AX.X),
                         (e32.name,), (b_.name,))
                    P.op("dve", lambda e_: e_.max(out=m8[:], in_=b_[:]), (b_.name,), (m8.name,))
                    P.tt("dve", c_[:, 0:1], m8[:, 1:2], m8[:, 0:1], ALU.subtract)
                    P.act(c_[:, 1:2], c_[:, 0:1], AF.Exp)
                    P.ts("dve", c_[:, 1:2], c_[:, 1:2], 1.0, ALU.add)
                    P.op("dve", lambda e_: e_.reciprocal(out=c_[:, 2:3], in_=c_[:, 1:2]), (c_.name,), (c_.name,))
                    P.ts("dve", c_[:, 3:4], c_[:, 2:3], -1.0, ALU.mult, 1.0, ALU.add)
                    P.tt("dve", WT[:, ti, 0:1], c_[:, 2:3], a_[:, 3:4], ALU.mult)
                    P.tt("dve", WT[:, ti, 1:2], c_[:, 3:4], a_[:, 3:4], ALU.mult)
                    for k in range(2):
                        P.ts("dve", d_[:], b_[:], m8[:, k:k + 1], ALU.is_equal)
                        P.tt("dve", OHK[:, ti, k, :].rearrange("p (g e) -> p g e", g=4),
                             g4["oh"][:].unsqueeze(2).to_broadcast([128, 4, 8]),
                             d_[:].unsqueeze(1).to_broadcast([128, 4, 8]), ALU.mult)
                    P.tt("dve", OHS[:, ti, :], OHK[:, ti, 0, :], OHK[:, ti, 1, :], ALU.add)
                    pp_ = nps()
                    P.mm(pp_[:, 0:32], trib_s[:], OHS[:, ti, :])
                    P.tt("dve", PF[:, ti, :], pp_[:, 0:32], cnt[:], ALU.add)
                    pc_ = nps()
                    P.mm(pc_[:, 0:32], onesb[:], OHS[:, ti, :])
                    P.tt("dve", cnt[:], cnt[:], pc_[:, 0:32], ALU.add)
                cmpb = T("m_cmp", [128, 96, 32])
                nbk = T("m_nbk", [128, 32])
                pend = T("m_pend", [128, 32])
                pst = T("m_pst", [128, 32])
                ones32 = T("m_o32", [128, 32])
                P.memset("pool", ones32[:], 1.0)
                cv = cmpb[:, 0:64, :].rearrange("p k e -> p e k")
                P.tt("dve", cv, cnt[:].unsqueeze(2).to_broadcast([128, 32, 64]), thr[:].unsqueeze(1).to_broadcast([128, 32, 64]), ALU.is_gt)
                P.op("dve", lambda e_: e_.reduce_sum(out=nbk[:], in_=cv, axis=AX.X), (cmpb.name,), (nbk.name,))
                P.scan(pend[:], ones32[:], nbk[:], 0.0)
                P.tt("dve", pst[:], pend[:], nbk[:], ALU.subtract)
                P.ts("dve", pst[:], pst[:], 128.0, ALU.mult)
                P.ts("dve", pend[:], pend[:], 128.0, ALU.mult)
                P.tt("dve", cmpb[:], pend[:].unsqueeze(1).to_broadcast([128, 96, 32]), nv[:].unsqueeze(2).to_broadcast([128, 96, 32]), ALU.is_le)
                bke = T("m_bke", [128, 96])
                P.op("dve", lambda e_: e_.reduce_sum(out=bke[:], in_=cmpb[:], axis=AX.X), (cmpb.name,), (bke.name,))
                P.ts("dve", bke[:], bke[:], 31.0, ALU.min, 128.0, ALU.mult)
                P.ts("dve", bke[:], bke[:], pvt[:, 0:1], ALU.add)
                dup = T("m_dup", [128, 96])
                P.memset("pool", dup[:], 0.0)
                P.tt("dve", dup[:, 1:96], bke[:, 1:96], bke[:, 0:95], ALU.is_equal)
                P.ts("dve", dup[:], dup[:], 1.0e6, ALU.mult)
                P.tt("dve", bke[:], bke[:], dup[:], ALU.add)
                idxw = T("m_idxw", [128, 96], I32)
                P.cp("dve", idxw[:], bke[:])
                DEST = T("m_dest", [128, NT, 2])
                DESTi = T("m_desti", [128, NT, 2], I32)
                dtmp = T("m_dtmp", [128, 32])
                for ti in range(NT):
                    P.tt("dve", PF[:, ti, :], PF[:, ti, :], pst[:], ALU.add)
                    for k in range(2):
                        P.tt("dve", dtmp[:], PF[:, ti, :], OHK[:, ti, k, :], ALU.mult)
                        P.op("dve", lambda e_, ti=ti, k=k: e_.reduce_sum(out=DEST[:, ti, k:k + 1], in_=dtmp[:], axis=AX.X),
                             (dtmp.name,), (DEST.name,))
                P.cp("dve", DESTi[:], DEST[:])
                xb16 = [T("m_xb%d" % i, [128, 1024], BF16) for i in range(2)]
                for ti in range(NT):
                    x_ = xt32[ti % 2]
                    xb_ = xb16[ti % 2]
                    P.dma("sp", x_[:], (X1[ti * 128:(ti + 1) * 128, :], "x1_%d" % ti))
                    P.cp("act" if ti % 2 else "dve", xb_[:], x_[:])
                    for k in range(2):
                        P.scatter((XS[0:NB * 128, :], "xs"), xb_[:], DESTi[:, ti, k:k + 1])
                wgu_f = T("m_wgu", [128, 4096], BF16)
                wdt_f = T("m_wd", [128, 2048], BF16)
                wgu1 = wgu_f[:].rearrange("p (s k c) -> p s k c", s=2, k=8)
                wdt1 = wdt_f[:].rearrange("p (f c) -> p f c", f=2)
                xbT = [T("m_xbT%d" % i, [128, 8, 128], BF16) for i in range(2)]
                sgl = [T("m_sg%d" % i, [128, 2, 128], BF16) for i in range(2)]
                hid = [T("m_hid%d" % i, [128, 2, 128], BF16) for i in range(2)]
                yo = [T("m_yo%d" % i, [128, 1024]) for i in range(2)]
                WG2 = WGUB[l]
                WD2 = WDB[l]
                for n in range(NB):
                    j = n % 2
                    xb_ = xb16[j]
                    P.dma("sp", xb_[:], (XS[n * 128:(n + 1) * 128, :], "xs"))
                    P.gather(wgu_f[:], (WG2, "wgub%d" % l), idxw[:, n:n + 1], bound=4095)
                    P.gather(wdt_f[:], (WD2, "wdb%d" % l), idxw[:, n:n + 1], bound=4095)
                    pb = PSB[j]
                    for kc in range(8):
                        P.tr(pb[:, kc * 128:(kc + 1) * 128], xb_[:, kc * 128:(kc + 1) * 128], identb[:])
                    P.cp("dve", xbT[j][:], pb[:].rearrange("p (k t) -> p k t", k=8))
                    pgu = nps()
                    pguv = pgu[:].rearrange("p (a t) -> p a t", a=4)
                    for s_ in range(2):
                        for ft in range(2):
                            for kc in range(8):
                                P.mm(pguv[:, s_ * 2 + ft, :], wgu1[:, s_, kc, ft * 128:(ft + 1) * 128], xbT[j][:, kc, :],
                                     start=(kc == 0 and s_ == 0 and ft == 0), stop=(kc == 7), skip_group_check=True)
                    P.act(sgl[j][:], pguv[:, 0:2, :], AF.Silu)
                    P.tt("dve", hid[j][:], pguv[:, 2:4, :], sgl[j][:], ALU.mult)
                    for hf in range(2):
                        py_ = nps()
                        for fc in range(2):
                            P.mm(py_[:], hid[j][:, fc, :], wdt1[:, fc, hf * 512:(hf + 1) * 512], start=(fc == 0), stop=(fc == 1))
                        P.cp("act" if hf else "dve", yo[j][:, hf * 512:(hf + 1) * 512], py_[:])
                    P.dma("sp", (YBd[n * 128:(n + 1) * 128, :], "ybd"), yo[j][:])
                g01 = [T("m_g%d" % i, [128, 1024]) for i in range(2)]
                lst2 = T("m_lst", [128, 4])
                for ti in range(NT):
                    x_ = xt32[ti % 2]
                    P.dma("sp", x_[:], (X1[ti * 128:(ti + 1) * 128, :], "x1_%d" % ti))
                    for k in range(2):
                        P.gather(g01[k][:], (YBd[0:NB * 128, :], "ybd"), DESTi[:, ti, k:k + 1])
                    P.act(x_[:], x_[:], AF.Copy, scale=float(ALPHA))
                    P.stt("dve", x_[:], g01[0][:], WT[:, ti, 0:1], x_[:], ALU.mult, ALU.add)
                    P.stt("dve", x_[:], g01[1][:], WT[:, ti, 1:2], x_[:], ALU.mult, ALU.add)
                    layer_norm(x_, g01[0], lst2, lnw2, lnb2)
                    if last:
                        P.dma("sp", (OUT[ti * 128:(ti + 1) * 128, :], "out"), x_[:])
                    else:
                        P.dma("sp", (X2[ti * 128:(ti + 1) * 128, :], "xres_%d" % ti), x_[:])
                        xb_ = xb16[ti % 2]
                        P.cp("act", xb_[:], x_[:])
                        pb = PSB[ti % 2]
                        for kc in range(8):
                            P.tr(pb[:, kc * 128:(kc + 1) * 128], xb_[:, kc * 128:(kc + 1) * 128], identb[:])
                        P.cp("dve", xbT[ti % 2][:], pb[:].rearrange("p (k t) -> p k t", k=8))
                        P.dma("sp", (XTd[:, :, 1 + ti * 128: 1 + (ti + 1) * 128], "XT_%d" % ti), xbT[ti % 2][:])
                    if debug and l == 0 and ti == dbg_blk and "X2" in dbg:
                        P.dma("sp", dbg["X2"], x_[:])
                P.barrier(bar[:])
        stats = P.finish()
        print("prog stats", stats)
    return nc


def kernel(**inputs):
    inp = {k: np.asarray(v) for k, v in inputs.items()}
    sh = prep_shared(inp)
    nc = bass.Bass("TRN2", target_bir_lowering=False)
    build(nc)
    shared = {k: np.ascontiguousarray(sh[k], dtype=np.float32) for k in IN_SHAPES if k != "x"}
    in_maps = []
    for c in range(8):
        m = dict(shared)
        m["x"] = np.ascontiguousarray(inp["x"][c % 4], dtype=np.float32)
        in_maps.append(m)
    res = run_bass_kernel_spmd(nc, in_maps, core_ids=list(range(8)))
    out = np.stack([np.asarray(res.results[b]["out"]) for b in range(4)]).astype(np.float32)
    return out
```

```python
import math
import numpy as np
from contextlib import ExitStack
import concourse.bass as bass
import concourse.mybir as mybir
from concourse.bass_utils import run_bass_kernel_spmd

F32 = mybir.dt.float32
BF16 = mybir.dt.bfloat16
I32 = mybir.dt.int32
ALU = mybir.AluOpType
AF = mybir.ActivationFunctionType
AX = mybir.AxisListType

COMPUTE = ("pe", "act", "dve", "pool")
NSLOT = 12

L_SEQ = 4096
DM = 1024
DEPTH = 4
ALPHA = (2 * DEPTH) ** 0.25
DIN = 6912
TB = 128
NBLK = L_SEQ // TB


def _k(x):
    if isinstance(x, tuple):
        ap, key = x
        if isinstance(key, int):
            key = "%s#%d" % (ap.name, key)
        return ap, key
    return x, x.name


class Prog:
    def __init__(self, nc, stack):
        self.nc = nc
        self.stack = stack
        self.eng = {"pe": nc.tensor, "act": nc.scalar, "dve": nc.vector,
                    "pool": nc.gpsimd, "sp": nc.sync}
        self.ins = []
        self.last_w = {}
        self.readers = {}
        self.last_barrier = None
        self.since_barrier = {}
        self.dmas_since = []
        self.psn = 0
        self.subs = {}
        self.bregs = {}

    def sb(self, name, shape, dt=F32, stack=None):
        self.nsb = getattr(self, "nsb", 0) + 1
        name = "%s_t%d" % (name, self.nsb)
        return (stack or self.stack).enter_context(self.nc.sbuf_tensor(name, list(shape), dt))

    def ps(self, name, shape, dt=F32, stack=None):
        return (stack or self.stack).enter_context(self.nc.psum_tensor(name, list(shape), dt))

    def dram(self, name, shape, dt=F32, kind="Internal"):
        return self.nc.dram_tensor(name, list(shape), dt, kind=kind).ap()

    def declare(self, tile, n):
        self.subs[tile.name] = ["%s#%d" % (tile.name, i) for i in range(n)]
        return tile

    def _expand(self, keys):
        out = []
        for k in keys:
            if k in self.subs:
                out.extend(self.subs[k])
            else:
                out.append(k)
        return out

    def _rec(self, eng, fn, reads, writes, dma):
        reads = self._expand(reads)
        writes = self._expand(writes)
        i = len(self.ins)
        deps = set()
        for k in reads:
            w = self.last_w.get(k)
            if w is not None:
                deps.add(w)
        for k in writes:
            w = self.last_w.get(k)
            if w is not None:
                deps.add(w)
            for r in self.readers.get(k, ()):
                deps.add(r)
        if self.last_barrier is not None:
            deps.add(self.last_barrier)
        deps.discard(i)
        for k in reads:
            self.readers.setdefault(k, []).append(i)
        for k in writes:
            self.last_w[k] = i
            self.readers[k] = []
        self.ins.append(dict(eng=eng, fn=fn, deps=deps, dma=dma))
        if dma:
            self.dmas_since.append(i)
        else:
            self.since_barrier[eng] = i
        return i

    def op(self, eng, fn, reads=(), writes=()):
        return self._rec(eng, fn, tuple(reads), tuple(writes), False)

    def barrier(self, scratch):
        deps = set(self.since_barrier.values()) | set(self.dmas_since)
        if self.last_barrier is not None:
            deps.add(self.last_barrier)
        i = len(self.ins)
        self.ins.append(dict(eng="dve", fn=lambda e: e.memset(scratch, 0.0), deps=deps, dma=False, barrier=True))
        self.last_barrier = i
        self.since_barrier = {"dve": i}
        self.dmas_since = []
        self.last_w = {}
        self.readers = {}
        return i

    def dma(self, q, out, in_, **kw):
        o, ok = _k(out)
        a, ak = _k(in_)
        return self._rec(q, lambda e: e.dma_start(out=o, in_=a, **kw), (ak,), (ok,), True)

    def dma_fn(self, q, fn, reads=(), writes=()):
        return self._rec(q, fn, tuple(reads), tuple(writes), True)

    def gather(self, out, src, idx, bound=None):
        o, ok = _k(out)
        s, sk = _k(src)
        ix, ik = _k(idx)
        rk = (sk, ik)
        if bound is None:
            return self._rec("pool", lambda e: e.indirect_dma_start(
                out=o, out_offset=None, in_=s, in_offset=bass.IndirectOffsetOnAxis(ap=ix, axis=0)),
                rk, (ok,), True)
        rk = (sk, ik, ok)

        def fn(e):
            if bound not in self.bregs:
                self.bregs[bound] = e.to_reg(bound)
            return e.indirect_dma_start(out=o, out_offset=None, in_=s,
                                        in_offset=bass.IndirectOffsetOnAxis(ap=ix, axis=0),
                                        bounds_check=self.bregs[bound], oob_is_err=False)
        return self._rec("pool", fn, rk, (ok,), True)

    def scatter(self, dst, src, idx):
        o, ok = _k(dst)
        s, sk = _k(src)
        ix, ik = _k(idx)
        return self._rec("pool", lambda e: e.indirect_dma_start(
            out=o, out_offset=bass.IndirectOffsetOnAxis(ap=ix, axis=0), in_=s, in_offset=None),
            (sk, ik), (ok,), True)

    def mm(self, out, lhsT, rhs, start=True, stop=True, xr=(), **kw):
        o, ok = _k(out)
        a, ak = _k(lhsT)
        b, bk = _k(rhs)
        return self.op("pe", lambda e: e.matmul(o, lhsT=a, rhs=b, start=start, stop=stop, **kw),
                       (ak, bk) + tuple(xr), (ok,))

    def tr(self, out, in_, ident):
        o, ok = _k(out)
        a, ak = _k(in_)
        b, bk = _k(ident)
        return self.op("pe", lambda e: e.transpose(out=o, in_=a, identity=b), (ak, bk), (ok,))

    def act(self, out, in_, func, bias=None, scale=None, accum=None, eng="act"):
        o, ok = _k(out)
        a, ak = _k(in_)
        rk = [ak]
        kw = {}
        if bias is not None:
            if isinstance(bias, (int, float)):
                kw["bias"] = float(bias)
            else:
                b, bk = _k(bias)
                kw["bias"] = b
                rk.append(bk)
        if scale is not None:
            if isinstance(scale, (int, float)):
                kw["scale"] = float(scale)
            else:
                s, sk = _k(scale)
                kw["scale"] = s
                rk.append(sk)
        wk = [ok]
        if accum is not None:
            c, ck = _k(accum)
            kw["accum_out"] = c
            wk.append(ck)
        return self.op("act", lambda e: e.activation(out=o, in_=a, func=func, **kw), rk, wk)

    def tt(self, eng, out, a, b, op):
        o, ok = _k(out)
        x, xk = _k(a)
        y, yk = _k(b)
        return self.op(eng, lambda e: e.tensor_tensor(out=o, in0=x, in1=y, op=op), (xk, yk), (ok,))

    def ts(self, eng, out, a, s1, op0, s2=None, op1=None, accum=None):
        o, ok = _k(out)
        x, xk = _k(a)
        rk = [xk]

        def sc(s):
            if s is None or isinstance(s, (int, float)):
                return None if s is None else float(s)
            p, pk = _k(s)
            rk.append(pk)
            return p
        v1 = sc(s1)
        v2 = sc(s2)
        kw = {}
        wk = [ok]
        if op1 is not None:
            kw["op1"] = op1
        if accum is not None:
            c, ck = _k(accum)
            kw["accum_out"] = c
            wk.append(ck)
        return self.op(eng, lambda e: e.tensor_scalar(out=o, in0=x, scalar1=v1, scalar2=v2, op0=op0, **kw),
                       rk, wk)

    def stt(self, eng, out, in0, scalar, in1, op0, op1):
        o, ok = _k(out)
        x, xk = _k(in0)
        y, yk = _k(in1)
        rk = [xk, yk]
        if isinstance(scalar, (int, float)):
            sv = float(scalar)
        else:
            sv, sk = _k(scalar)
            rk.append(sk)
        return self.op(eng, lambda e: e.scalar_tensor_tensor(out=o, in0=x, scalar=sv, in1=y, op0=op0, op1=op1),
                       rk, (ok,))

    def cp(self, eng, out, in_):
        o, ok = _k(out)
        a, ak = _k(in_)
        if eng == "act":
            return self.op("act", lambda e: e.copy(out=o, in_=a), (ak,), (ok,))
        return self.op(eng, lambda e: e.tensor_copy(out=o, in_=a), (ak,), (ok,))

    def memset(self, eng, out, val):
        o, ok = _k(out)
        return self.op(eng, lambda e: e.memset(o, val), (), (ok,))

    def scan(self, out, d0, d1, init, op0=ALU.mult, op1=ALU.add):
        o, ok = _k(out)
        a, ak = _k(d0)
        b, bk = _k(d1)
        rk = [ak, bk]
        if isinstance(init, (int, float)):
            iv = float(init)
        else:
            iv, ik = _k(init)
            rk.append(ik)
        return self.op("dve", lambda e: e.tensor_tensor_scan(out=o, data0=a, data1=b, initial=iv, op0=op0, op1=op1),
                       rk, (ok,))

    def finish(self, final_wait_eng="sp"):
        nc = self.nc
        ins = self.ins
        n = len(ins)

        def skip(src, it):
            return src["eng"] == "pe" and it["eng"] == "pe" and not src["dma"] and not it["dma"]
        need = [False] * n
        for i, it in enumerate(ins):
            for d in it["deps"]:
                if not skip(ins[d], it):
                    need[d] = True
        sems = {e: self.stack.enter_context(nc.semaphore("s_" + e)) for e in COMPUTE}
        qs = sorted({it["eng"] for it in ins if it["dma"]})
        dsem = {q: [self.stack.enter_context(nc.semaphore("d_%s_%d" % (q, j)))
                    for j in range(NSLOT)] for q in qs}
        cnt = {e: 0 for e in COMPUTE}
        dcnt = {q: 0 for q in qs}
        slot_uses = {q: [0] * NSLOT for q in qs}
        sig = [None] * n
        waited = {}
        nwait = 0
        epoch = 0
        for i, it in enumerate(ins):
            e = it["eng"]
            eo = self.eng[e]
            want = {}
            for d in it["deps"]:
                src = ins[d]
                if skip(src, it):
                    continue
                s, v = sig[d]
                key = id(s)
                if key not in want or want[key][1] < v:
                    want[key] = (s, v)
            if it["dma"]:
                j = dcnt[e] % NSLOT
                if slot_uses[e][j] > 0:
                    s = dsem[e][j]
                    v = 16 * slot_uses[e][j]
                    key = id(s)
                    if key not in want or want[key][1] < v:
                        want[key] = (s, v)
            for key, (s, v) in want.items():
                wk = (e, key)
                if waited.get(wk, -1) >= v:
                    continue
                eo.wait_ge(s, v)
                nwait += 1
                waited[wk] = v
            bi = it["fn"](eo)
            if it["dma"]:
                j = dcnt[e] % NSLOT
                slot_uses[e][j] += 1
                dcnt[e] += 1
                bi.then_inc(dsem[e][j], 16)
                sig[i] = (dsem[e][j], 16 * slot_uses[e][j])
            elif need[i]:
                cnt[e] += 1
                bi.then_inc(sems[e], 1)
                sig[i] = (sems[e], cnt[e])
            if it.get("barrier"):
                epoch += 1
                sems = {e2: self.stack.enter_context(nc.semaphore("s%d_%s" % (epoch, e2))) for e2 in COMPUTE}
                cnt = {e2: 0 for e2 in COMPUTE}
        fe = self.eng[final_wait_eng]
        for q in qs:
            for j in range(NSLOT):
                if slot_uses[q][j] > 0:
                    fe.wait_ge(dsem[q][j], 16 * slot_uses[q][j])
        self.stats = dict(n=n, cnt=dict(cnt), dcnt=dict(dcnt), nwait=nwait)
        return self.stats


def fm(a, nt):
    return np.ascontiguousarray(np.asarray(a).reshape(nt, 128).T)


def prep_shared(inp):
    f = np.float32
    d = {}
    d["w_in"] = np.ascontiguousarray(inp["w_in"], dtype=f)
    d["mu_fm"] = np.stack([fm(inp["rwkv_mu"][l], 14) for l in range(DEPTH)]).astype(f)
    d["gate_b_fm"] = np.stack([fm(inp["gate_b"][l], 24) for l in range(DEPTH)]).astype(f)
    d["ident"] = np.eye(128, dtype=f)
    are, aim, ldt = inp["ssm_a_re"], inp["ssm_a_im"], inp["ssm_log_dt"]
    s5fm = np.zeros((DEPTH, 128, 3, 16), f)
    for st in range(16):
        for gg in range(2):
            g = 2 * st + gg
            s5fm[:, gg * 64:(gg + 1) * 64, 0, st] = are[:, g, :]
            s5fm[:, gg * 64:(gg + 1) * 64, 1, st] = aim[:, g, :]
            s5fm[:, gg * 64:(gg + 1) * 64, 2, st] = ldt[:, g, None]
    d["s5fm"] = s5fm
    bre, bim = inp["ssm_b_re"], inp["ssm_b_im"]
    s5row = np.zeros((DEPTH, 128, 4, 5, 64), f)
    for kc in range(4):
        for g8 in range(8):
            g = 8 * kc + g8
            rs = slice(g8 * 16, g8 * 16 + 16)
            s5row[:, rs, kc, 0, :] = are[:, g, None, :]
            s5row[:, rs, kc, 1, :] = aim[:, g, None, :]
            s5row[:, rs, kc, 2, :] = ldt[:, g, None, None]
            s5row[:, rs, kc, 3, :] = bre[:, g].transpose(0, 2, 1)
            s5row[:, rs, kc, 4, :] = bim[:, g].transpose(0, 2, 1)
    d["s5row"] = s5row
    cre, cim = inp["ssm_c_re"], inp["ssm_c_im"]
    s5c = np.zeros((DEPTH, 128, 2, 16, 16), f)
    for st in range(16):
        for gg in range(2):
            g = 2 * st + gg
            s5c[:, gg * 64:(gg + 1) * 64, 0, st, :] = cre[:, g].transpose(0, 2, 1)
            s5c[:, gg * 64:(gg + 1) * 64, 1, st, :] = cim[:, g].transpose(0, 2, 1)
    d["s5c"] = s5c
    rowmask = np.zeros((128, 8), f)
    for g8 in range(8):
        rowmask[g8 * 16:(g8 + 1) * 16, g8] = 1
    halfmask = np.zeros((128, 2), f)
    halfmask[:64, 0] = 1
    halfmask[64:, 1] = 1
    d["rowmask"] = rowmask
    d["halfmask"] = halfmask
    d["tvals"] = np.tile(np.arange(1, 129, dtype=f)[None, :], (128, 1))
    d["ssm_d_fm"] = np.stack([fm(inp["ssm_d"][l], 4) for l in range(DEPTH)]).astype(f)
    d["glu_b_fm"] = np.stack([fm(inp["ssm_glu_b"][l], 8) for l in range(DEPTH)]).astype(f)
    d["glu_w"] = np.ascontiguousarray(inp["ssm_glu_w"], dtype=f)
    import ml_dtypes
    slopes = (2.0 ** (-(np.arange(1, 9, dtype=np.float64)))).astype(f)
    aj = slopes[:, None] * np.arange(256, dtype=f)[None, :]
    hi = aj.astype(ml_dtypes.bfloat16).astype(f)
    lo = (aj - hi).astype(ml_dtypes.bfloat16).astype(f)
    d["alibi_hl"] = np.stack([hi, lo]).astype(f)
    p = np.arange(128, dtype=f)[:, None, None]
    n = np.arange(16, dtype=f)[None, None, :]
    d["mb_t1"] = (-slopes[None, :, None] * (p - 256.0 * n)).astype(f)
    d["mb_slopes"] = np.tile(slopes[None, :], (128, 1)).astype(f)
    tri = np.zeros((128, 128), f)
    tri[np.arange(128)[:, None] < np.arange(128)[None, :]] = -30000.0
    d["mb_tri"] = tri
    rv = np.zeros((DEPTH, 128, 5, 4), f)
    for l in range(DEPTH):
        rv[l, :, 0, :] = fm(inp["rwkv_w0"][l], 4)
        rv[l, :, 1, :] = fm(inp["rwkv_a0"][l], 4)
        rv[l, :, 2, :] = fm(inp["rwkv_k_k"][l], 4)
        rv[l, :, 3, :] = fm(inp["rwkv_k_a"][l], 4)
        rv[l, :, 4, :] = fm(inp["rwkv_r_k"][l].reshape(512), 4)
    d["rw_vecs"] = rv
    d["rw_w2a2"] = np.concatenate([inp["rwkv_w2"], inp["rwkv_a2"]], axis=1).astype(f)
    d["rw_g2"] = np.ascontiguousarray(inp["rwkv_g2"], dtype=f)
    d["rw_ln"] = np.stack([np.tile(inp["rwkv_ln_w"][:, None, :], (1, 64, 1)),
                           np.tile(inp["rwkv_ln_b"][:, None, :], (1, 64, 1))], axis=2).astype(f)
    tt_ = np.arange(64)
    ml = (tt_[None, :] < tt_[:, None]).astype(f)
    mu_ = (tt_[None, :] > tt_[:, None]).astype(f)
    mui = (tt_[None, :] >= tt_[:, None]).astype(f)
    d["rw_masks"] = np.stack([ml, mu_, mui], axis=1).astype(f)
    bo = np.zeros((128, 128), f)
    bo[:64, :64] = 1
    bo[64:, 64:] = 1
    d["rw_bo"] = bo
    d["w_up"] = np.stack([inp["w_up_rwkv"], inp["w_up_moba"], inp["w_up_ssm"]], axis=1).astype(f)
    d["w_out"] = np.ascontiguousarray(inp["w_out"], dtype=f)
    d["ln1_rep"] = np.stack([np.tile(inp["ln1_w"][:, None, :], (1, 128, 1)),
                             np.tile(inp["ln1_b"][:, None, :], (1, 128, 1))], axis=1).astype(f)
    rw = np.concatenate([inp["router_group_w"], inp["router_expert_w"]], axis=2).astype(f)
    d["moe_rw"] = np.ascontiguousarray(rw.reshape(DEPTH, 8, 128, 36).transpose(0, 2, 1, 3))
    rb = np.concatenate([inp["router_group_b"], inp["router_expert_b"]], axis=1).astype(f)
    d["moe_rb"] = np.ascontiguousarray(np.tile(rb[:, None, :], (1, 128, 1)))
    wg = inp["expert_w_gate"].reshape(DEPTH, 32, 8, 128, 256).transpose(0, 1, 3, 2, 4)
    wu = inp["expert_w_up"].reshape(DEPTH, 32, 8, 128, 256).transpose(0, 1, 3, 2, 4)
    d["moe_wgu"] = np.ascontiguousarray(np.stack([wg, wu], axis=3).reshape(DEPTH, 32 * 128, 4096), dtype=f)
    wd = inp["expert_w_down"].reshape(DEPTH, 32, 2, 128, 1024).transpose(0, 1, 3, 2, 4)
    d["moe_wd"] = np.ascontiguousarray(wd.reshape(DEPTH, 32 * 128, 2048), dtype=f)
    tri_s = (np.arange(128)[:, None] < np.arange(128)[None, :]).astype(f)
    d["moe_tri"] = tri_s
    d["moe_thr"] = np.tile((128.0 * np.arange(64, dtype=f))[None, :], (128, 1))
    d["moe_nv"] = np.tile((128.0 * np.arange(96, dtype=f))[None, :], (128, 1))
    d["moe_pv"] = np.arange(128, dtype=f)[:, None].copy()
    d["ln2_rep"] = np.stack([np.tile(inp["ln2_w"][:, None, :], (1, 128, 1)),
                             np.tile(inp["ln2_b"][:, None, :], (1, 128, 1))], axis=1).astype(f)
    return d


DBG = {}
RWCUT = 9
SKIP = set()
NPSF = 6
PI = math.pi

IN_SHAPES = {
    "x": [L_SEQ, DM], "w_in": [DEPTH, DM, DIN], "mu_fm": [DEPTH, 128, 14], "gate_b_fm": [DEPTH, 128, 24],
    "ident": [128, 128], "s5fm": [DEPTH, 128, 3, 16], "s5row": [DEPTH, 128, 4, 5, 64],
    "s5c": [DEPTH, 128, 2, 16, 16], "rowmask": [128, 8], "halfmask": [128, 2], "tvals": [128, 128],
    "ssm_d_fm": [DEPTH, 128, 4], "glu_b_fm": [DEPTH, 128, 8], "glu_w": [DEPTH, 512, 1024],
    "alibi_hl": [2, 8, 256], "mb_t1": [128, 8, 16], "mb_slopes": [128, 8], "mb_tri": [128, 128],
    "rw_vecs": [DEPTH, 128, 5, 4], "rw_w2a2": [DEPTH, 128, 512], "rw_g2": [DEPTH, 128, 512],
    "rw_ln": [DEPTH, 64, 2, 512], "rw_masks": [64, 3, 64], "rw_bo": [128, 128],
    "w_up": [DEPTH, 3, 512, 1024], "w_out": [DEPTH, 1024, 1024],
    "ln1_rep": [DEPTH, 2, 128, 1024], "ln2_rep": [DEPTH, 2, 128, 1024],
    "moe_rw": [DEPTH, 128, 8, 36], "moe_rb": [DEPTH, 128, 36], "moe_wgu": [DEPTH, 4096, 4096],
    "moe_wd": [DEPTH, 4096, 2048], "moe_tri": [128, 128], "moe_thr": [128, 64], "moe_nv": [128, 96],
    "moe_pv": [128, 1],
}


def build(nc, n_layers=DEPTH, debug=False, nblk=NBLK, dbg_blk=1):
    D = {k: nc.dram_tensor(k, v, F32, kind="ExternalInput").ap() for k, v in IN_SHAPES.items()}
    OUT = nc.dram_tensor("out", [L_SEQ, DM], F32, kind="ExternalOutput").ap()
    dbg = {}
    if debug:
        for nm, shp in DBG.items():
            dbg[nm] = nc.dram_tensor("dbg_" + nm, list(shp), F32, kind="ExternalOutput").ap()

    with ExitStack() as st:
        P = Prog(nc, st)
        XTd = P.dram("xtd", [128, 8, L_SEQ + 1], BF16)
        identf = P.sb("identf", [128, 128], F32)
        identb = P.sb("identb", [128, 128], BF16)
        rowmask = P.sb("rowmask", [128, 8], F32)
        halfmask = P.sb("halfmask", [128, 2], F32)
        tvals = P.sb("tvals", [128, 128], F32)
        bar = P.sb("bar", [128, 1], F32)
        PSF = [P.ps("psf%d" % i, [128, 512], F32) for i in range(NPSF)]
        PSB = [P.ps("psb%d" % i, [128, 1024], BF16) for i in range(2)]

        def nps():
            t = PSF[P.psn % 4]
            P.psn += 1
            return t

        P.dma("sp", identf[:], D["ident"][:, :])
        P.dma("sp", rowmask[:], D["rowmask"][:, :])
        P.dma("sp", halfmask[:], D["halfmask"][:, :])
        P.dma("sp", tvals[:], D["tvals"][:, :])
        P.cp("dve", identb[:], identf[:])
        ALR = P.sb("ALR", [128, 8, 256], BF16)
        ones2 = P.sb("ones2", [128, 128], BF16)
        mbT1 = P.sb("mbT1", [128, 8, 16], F32)
        mbSl = P.sb("mbSl", [128, 8], F32)
        trib = P.sb("trib", [128, 128], BF16)
        P.memset("pool", ones2[:], 1.0)
        P.dma("sp", mbT1[:], D["mb_t1"][:, :, :])
        P.dma("sp", mbSl[:], D["mb_slopes"][:, :])
        rwmask = P.sb("rwmask", [64, 3, 64], F32)
        BOb = P.sb("BOb", [128, 128], BF16)
        ones64 = P.sb("ones64", [128, 64], F32)
        P.dma("sp", rwmask[:], D["rw_masks"][:, :, :])
        P.memset("pool", ones64[:], 1.0)
        KTd = P.dram("ktd", [16, 128, 4, 256], BF16)
        Vd = P.dram("vd", [16, 128, 2, 8 * 65], BF16)

        WINB = P.dram("winb", [DEPTH, DM, DIN], BF16)
        GLUB = P.dram("glub", [DEPTH, 512, 1024], BF16)
        UPB = P.dram("upb", [DEPTH, 3, 512, 1024], BF16)
        WOB = P.dram("wob", [DEPTH, 1024, 1024], BF16)
        WGUB = [P.dram("wgub%d" % i, [4096, 4096], BF16) for i in range(DEPTH)]
        WDB = [P.dram("wdb%d" % i, [4096, 2048], BF16) for i in range(DEPTH)]
        XS = P.dram("xsort", [96 * 128, DM], BF16)
        YBd = P.dram("ybd", [96 * 128, DM], F32)
        X1 = P.dram("x1s", [L_SEQ, DM], F32)
        X2 = P.dram("x2s", [L_SEQ, DM], F32)
        for l in range(n_layers):
            for r0 in range(0, DM, 256):
                P.dma("pool", (WINB[l, r0:r0 + 256, :], "winb%d" % l), (D["w_in"][l, r0:r0 + 256, :], "w_in"))
            P.dma("pool", (GLUB[l], "glub%d" % l), (D["glu_w"][l], "glu_w"))
            for br in range(3):
                P.dma("pool", (UPB[l, br], "upb%d" % l), (D["w_up"][l, br], "w_up"))
            P.dma("pool", (WOB[l], "wob%d" % l), (D["w_out"][l], "w_out"))
            for r0 in range(0, 4096, 512):
                P.dma("pool", (WGUB[l][r0:r0 + 512, :], "wgub%d" % l), (D["moe_wgu"][l, r0:r0 + 512, :], "moe_wgu"))
            for r0 in range(0, 4096, 1024):
                P.dma("pool", (WDB[l][r0:r0 + 1024, :], "wdb%d" % l), (D["moe_wd"][l, r0:r0 + 1024, :], "moe_wd"))

        with ExitStack() as s0:
            alr32 = P.sb("alr32", [128, 8, 256], F32, s0)
            tri32 = P.sb("tri32", [128, 128], F32, s0)
            bo32 = P.sb("bo32", [128, 128], F32, s0)
            P.memset("pool", alr32[:], 0.0)
            P.dma("sp", alr32[0:2], D["alibi_hl"][:, :, :])
            P.dma("sp", alr32[64:66], D["alibi_hl"][:, :, :])
            P.cp("dve", ALR[:], alr32[:])
            P.dma("sp", tri32[:], D["mb_tri"][:, :])
            P.cp("dve", trib[:], tri32[:])
            P.dma("sp", bo32[:], D["rw_bo"][:, :])
            P.cp("dve", BOb[:], bo32[:])
            zt = P.sb("zt", [128, DM], BF16, s0)
            P.memset("pool", zt[:], 0.0)
            for n in range(2 * nblk + 32):
                P.dma("sp", (XS[n * 128:(n + 1) * 128, :], "xs"), zt[:])
            xin = [P.sb("xin%d" % i, [128, DM], F32, s0) for i in range(2)]
            xinb = [P.sb("xinb%d" % i, [128, DM], BF16, s0) for i in range(2)]
            xz = P.sb("xz", [128, 8, 1], BF16, s0)
            xst = [P.sb("xst%d" % i, [128, 8, 128], BF16, s0) for i in range(2)]
            P.memset("pool", xz[:], 0.0)
            P.dma("sp", (XTd[:, :, 0:1], "XTz"), xz[:], allow_slow_non_contiguous=True)
            for ti in range(0 if 'xload' in SKIP else L_SEQ // 128):
                h2 = ti % 2
                xi, xb, pb = xin[h2], xinb[h2], PSB[h2]
                P.dma("sp", xi[:], (D["x"][ti * 128:(ti + 1) * 128, :], "x"))
                P.cp("act" if h2 else "dve", xb[:], xi[:])
                for kc in range(8):
                    P.tr(pb[:, kc * 128:(kc + 1) * 128], xb[:, kc * 128:(kc + 1) * 128], identb[:])
                P.cp("dve" if h2 else "act", xst[h2][:], pb[:].rearrange("p (k t) -> p k t", k=8))
                P.dma("sp", (XTd[:, :, 1 + ti * 128: 1 + (ti + 1) * 128], "XT_%d" % ti), xst[h2][:])
            P.barrier(bar[:])

        for l in range(n_layers):
            with ExitStack() as sm:
                def T(name, shape, dt=F32):
                    return P.sb(name, shape, dt, sm)
                mu = T("mu", [128, 14])
                omu = T("omu", [128, 14])
                gateb = T("gateb", [128, 24])
                P.dma("sp", mu[:], D["mu_fm"][l])
                P.dma("sp", gateb[:], D["gate_b_fm"][l])
                P.ts("dve", omu[:], mu[:], -1.0, ALU.mult, 1.0, ALU.add)
                hgateb = T("hgateb", [128, 24])
                P.ts("dve", hgateb[:], gateb[:], 0.5, ALU.mult)
                wbuf = [T("wbuf%d" % i, [128, 8, 256], BF16) for i in range(3)]
                wctr = [0]

                def nwb():
                    w = wbuf[wctr[0] % 3]
                    wctr[0] += 1
                    return w
                RW = P.declare(T("RW", [128, 14, TB]), 14)
                tmpA = [T("tmpA%d" % i, [128, TB]) for i in range(2)]
                QT32 = P.declare(T("QT32", [128, 4, TB]), 4)
                U32 = P.declare(T("U32", [128, 4, TB]), 4)
                Ub = P.declare(T("Ub", [128, 4, TB], BF16), 4)
                GS = P.declare(T("GS", [128, 24, TB], BF16), 24)
                WV = WINB[l].rearrange("(kc p) c -> p kc c", p=128)

                Ec = T("Ec", [128, 16, 128])
                Es = T("Es", [128, 16, 128])
                LB = T("LB", [128, 16, 2, 128], BF16)
                LC = T("LC", [128, 16, 2, 128], BF16)
                mag = T("mag", [128, 16])
                ssmd = T("ssmd", [128, 4])
                glub = T("glub", [128, 8])
                Hc = T("Hc", [128, 16, 2])
                P.dma("sp", ssmd[:], D["ssm_d_fm"][l])
                P.dma("sp", glub[:], D["glu_b_fm"][l])
                P.memset("pool", Hc[:], 0.0)
                with ExitStack() as stmp:
                    def TT(name, shape, dt=F32):
                        return P.sb(name, shape, dt, stmp)
                    wtmp = {}

                    def wrap(eng, out, x, off, shape):
                        key = (eng, tuple(shape))
                        if key not in wtmp:
                            wtmp[key] = (TT("wr_a", shape), TT("wr_i", shape, I32), TT("wr_b", shape))
                        xo, ki, kf = wtmp[key]
                        P.ts(eng, xo[:], x, float(off), ALU.add)
                        P.ts(eng, kf[:], xo[:], 1.0 / (2 * PI), ALU.mult)
                        P.cp(eng, ki[:], kf[:])
                        P.cp(eng, kf[:], ki[:])
                        P.ts(eng, kf[:], kf[:], -2 * PI, ALU.mult)
                        P.tt(eng, xo[:], xo[:], kf[:], ALU.add)
                        P.ts(eng, kf[:], xo[:], PI, ALU.is_gt, -2 * PI, ALU.mult)
                        P.tt(eng, out, xo[:], kf[:], ALU.add)

                    def s5_params(pfx, are, aim, ldt, shape):
                        t = {n: TT(pfx + n, shape) for n in ("dt", "mag", "th", "sn", "cs", "t1", "t2", "t3", "cre", "cim")}
                        P.act(t["dt"][:], ldt, AF.Exp)
                        P.tt("dve", t["t1"][:], are, t["dt"][:], ALU.mult)
                        P.act(t["mag"][:], t["t1"][:], AF.Exp)
                        P.tt("dve", t["th"][:], aim, t["dt"][:], ALU.mult)
                        wrap("dve", t["t1"][:], t["th"][:], 0.0, shape)
                        P.act(t["sn"][:], t["t1"][:], AF.Sin)
                        wrap("dve", t["t2"][:], t["th"][:], 0.5 * PI, shape)
                        P.act(t["cs"][:], t["t2"][:], AF.Sin)
                        lre, lim = t["t1"], t["t2"]
                        P.tt("dve", lre[:], t["mag"][:], t["cs"][:], ALU.mult)
                        P.tt("dve", lim[:], t["mag"][:], t["sn"][:], ALU.mult)
                        den = t["t3"]
                        P.tt("dve", den[:], are, are, ALU.mult)
                        P.tt("dve", t["cre"][:], aim, aim, ALU.mult)
                        P.tt("dve", den[:], den[:], t["cre"][:], ALU.add)
                        P.op("dve", lambda e: e.reciprocal(out=den[:], in_=den[:]), (den.name,), (den.name,))
                        P.ts("dve", lre[:], lre[:], -1.0, ALU.add)
                        P.tt("dve", t["cre"][:], lre[:], are, ALU.mult)
                        P.tt("dve", t["cim"][:], lim[:], aim, ALU.mult)
                        P.tt("dve", t["cre"][:], t["cre"][:], t["cim"][:], ALU.add)
                        P.tt("dve", t["cre"][:], t["cre"][:], den[:], ALU.mult)
                        P.tt("dve", t["cim"][:], lim[:], are, ALU.mult)
                        P.tt("dve", t["sn"][:], lre[:], aim, ALU.mult)
                        P.tt("dve", t["cim"][:], t["cim"][:], t["sn"][:], ALU.subtract)
                        P.tt("dve", t["cim"][:], t["cim"][:], den[:], ALU.mult)
                        return t

                    s5fm = TT("s5fm", [128, 3, 16])
                    P.dma("sp", s5fm[:], D["s5fm"][l])
                    pf = s5_params("f_", s5fm[:, 0, :], s5fm[:, 1, :], s5fm[:, 2, :], [128, 16])
                    P.cp("dve", mag[:], pf["mag"][:])
                    th = pf["th"]
                    angt = [TT("angt%d" % i, [128, 128]) for i in range(4)]
                    for s_ in range(16):
                        for j, (off, dst) in enumerate(((0.0, Es), (0.5 * PI, Ec))):
                            a = angt[j]
                            eng_ = "pool" if s_ % 2 else "dve"
                            a = angt[j + 2 * (s_ % 2)]
                            P.ts(eng_, a[:], tvals[:], th[:, s_:s_ + 1], ALU.mult)
                            wrap(eng_, a[:], a[:], off, [128, 128])
                            P.act((dst[:, s_, :], dst.name), a[:], AF.Sin)
                    s5row = TT("s5row", [128, 4, 5, 64])
                    P.dma("sp", s5row[:], D["s5row"][l])
                    pr = s5_params("r_", s5row[:, :, 0, :], s5row[:, :, 1, :], s5row[:, :, 2, :], [128, 4, 64])
                    bbr = TT("bbr", [128, 4, 64])
                    bbi = TT("bbi", [128, 4, 64])
                    tb1 = TT("tb1", [128, 4, 64])
                    P.tt("dve", bbr[:], pr["cre"][:], s5row[:, :, 3, :], ALU.mult)
                    P.tt("dve", tb1[:], pr["cim"][:], s5row[:, :, 4, :], ALU.mult)
                    P.tt("dve", bbr[:], bbr[:], tb1[:], ALU.subtract)
                    P.tt("dve", bbi[:], pr["cre"][:], s5row[:, :, 4, :], ALU.mult)
                    P.tt("dve", tb1[:], pr["cim"][:], s5row[:, :, 3, :], ALU.mult)
                    P.tt("dve", bbi[:], bbi[:], tb1[:], ALU.add)
                    for s_ in range(16):
                        kc = s_ // 4
                        for gg in range(2):
                            g8 = (2 * s_ + gg) % 8
                            for ri, src in enumerate((bbr, bbi)):
                                P.ts("pool", LB[:, s_, ri, gg * 64:(gg + 1) * 64], src[:, kc, :],
                                     rowmask[:, g8:g8 + 1], ALU.mult)
                    s5c = TT("s5c", [128, 2, 16, 16])
                    P.dma("sp", s5c[:], D["s5c"][l])
                    P.memset("pool", LC[:], 0.0)
                    for s_ in range(16):
                        for gg in range(2):
                            g8 = (2 * s_ + gg) % 8
                            P.ts("pool", LC[:, s_, 0, g8 * 16:(g8 + 1) * 16], s5c[:, 0, s_, :],
                                 halfmask[:, gg:gg + 1], ALU.mult)
                            P.ts("pool", LC[:, s_, 1, g8 * 16:(g8 + 1) * 16], s5c[:, 1, s_, :],
                                 halfmask[:, gg:gg + 1], ALU.mult, -1.0, ALU.mult)
                    P.barrier(bar[:])
                XTl = [T("s5x%d" % i, [128, 2, 2, 128]) for i in range(2)]
                S5t = [T("s5t%d" % i, [128, 2, 2, 128]) for i in range(1)]
                S5g = [T("s5g%d" % i, [128, 2, 2, 128]) for i in range(2)]
                S5h = [T("s5h%d" % i, [128, 2, 2, 128]) for i in range(1)]
                HB = [T("s5hb%d" % i, [128, 4, 2, 128], BF16) for i in range(2)]
                YG = T("YG", [128, 4, TB], BF16)
                GLb = [T("GLb%d" % i, [128, 4, 256], BF16) for i in range(2)]
                YC = T("YC", [128, 4, TB], BF16)
                SGT = T("SGT", [128, 4, TB])
                hglub = T("hglub", [128, 8])
                P.ts("dve", hglub[:], glub[:], 0.5, ALU.mult)

                def s5_block(blk):
                    for sg in range(8):
                        kc = sg // 2
                        ps = nps()
                        bu = ps[:].rearrange("p (a b t) -> p a b t", a=2, b=2)
                        for j in range(2):
                            s_ = 2 * sg + j
                            for ri in range(2):
                                P.mm(bu[:, j, ri, :], LB[:, s_, ri, :], (Ub[:, kc, :], kc))
                        xt_, tt_, g_, h_ = XTl[sg % 2], S5t[0], S5g[sg % 2], S5h[0]
                        ec = Ec[:, 2 * sg:2 * sg + 2, :]
                        es = Es[:, 2 * sg:2 * sg + 2, :]
                        P.tt("dve", xt_[:, :, 0, :], ec, bu[:, :, 0, :], ALU.mult)
                        P.tt("dve", tt_[:, :, 0, :], es, bu[:, :, 1, :], ALU.mult)
                        P.tt("dve", xt_[:, :, 1, :], ec, bu[:, :, 1, :], ALU.mult)
                        P.tt("dve", tt_[:, :, 1, :], es, bu[:, :, 0, :], ALU.mult)
                        P.tt("pool", xt_[:, :, 0, :], xt_[:, :, 0, :], tt_[:, :, 0, :], ALU.add)
                        P.tt("pool", xt_[:, :, 1, :], xt_[:, :, 1, :], tt_[:, :, 1, :], ALU.subtract)
                        for j in range(2):
                            s_ = 2 * sg + j
                            for ri in range(2):
                                P.scan(g_[:, j, ri, :], mag[:, s_:s_ + 1].to_broadcast([128, 128]), xt_[:, j, ri, :],
                                       Hc[:, s_, ri:ri + 1])
                        P.tt("pool", h_[:, :, 0, :], ec, g_[:, :, 0, :], ALU.mult)
                        P.tt("pool", tt_[:, :, 0, :], es, g_[:, :, 1, :], ALU.mult)
                        P.tt("pool", h_[:, :, 1, :], ec, g_[:, :, 1, :], ALU.mult)
                        P.tt("pool", tt_[:, :, 1, :], es, g_[:, :, 0, :], ALU.mult)
                        P.tt("pool", h_[:, :, 0, :], h_[:, :, 0, :], tt_[:, :, 0, :], ALU.subtract)
                        P.tt("pool", h_[:, :, 1, :], h_[:, :, 1, :], tt_[:, :, 1, :], ALU.add)
                        P.cp("act", Hc[:, 2 * sg:2 * sg + 2, :], h_[:, :, :, 127])
                        yield
                        hb = HB[kc % 2]
                        P.cp("act", hb[:, (sg % 2) * 2:(sg % 2) * 2 + 2, :, :], h_[:])
                        if sg % 2 == 1:
                            py = nps()
                            n = 0
                            for j in range(4):
                                s_ = kc * 4 + j
                                for ri in range(2):
                                    P.mm(py[:, 0:TB], LC[:, s_, ri, :], hb[:, j, ri, :], start=(n == 0), stop=(n == 7))
                                    n += 1
                            P.stt("dve", SGT[:, kc, :], (U32[:, kc, :], kc), ssmd[:, kc:kc + 1], py[:, 0:TB], ALU.mult, ALU.add)
                    P.act(YG[:], SGT[:], AF.Gelu)
                    yield
                    GV = GLUB[l].rearrange("(kc p) c -> p kc c", p=128)
                    for oc in range(4):
                        gl = GLb[oc % 2]
                        P.dma("sp", gl[:, :, 0:128], (GV[:, :, oc * 128:(oc + 1) * 128], "glub%d" % l))
                        P.dma("sp", gl[:, :, 128:256], (GV[:, :, (oc + 4) * 128:(oc + 5) * 128], "glub%d" % l))
                        p1 = nps()
                        p2 = nps()
                        for kc in range(4):
                            P.mm(p1[:, 0:TB], gl[:, kc, 0:128], YG[:, kc, :], start=(kc == 0), stop=(kc == 3))
                        for kc in range(4):
                            P.mm(p2[:, 0:TB], gl[:, kc, 128:256], YG[:, kc, :], start=(kc == 0), stop=(kc == 3))
                        sg_ = SGT[:, oc, :]
                        P.act(sg_, p2[:, 0:TB], AF.Tanh, bias=hglub[:, oc + 4:oc + 5], scale=0.5)
                        P.ts("pool", sg_, sg_, 0.5, ALU.mult, 0.5, ALU.add)
                        P.stt("dve", YC[:, oc, :], p1[:, 0:TB], glub[:, oc:oc + 1], sg_, ALU.add, ALU.mult)
                        yield


                QTb = P.declare(T("QTb", [128, 4, TB], BF16), 4)
                VTf = P.declare(T("VTf", [128, 4, TB], BF16), 4)
                KTb = P.declare(T("KTb", [128, 4, 256], BF16), 4)
                Vb = T("Vb", [128, 2, 8, 65], BF16)
                P.memset("pool", Vb[:], 1.0)
                ksum = T("ksum", [128, 4, 32])
                kmean = T("kmean", [128, 4, 16])
                KTs = [T("KTs%d" % i, [128, 4, 256], BF16) for i in range(2)]
                Vs = [T("Vs%d" % i, [128, 2, 8, 65], BF16) for i in range(2)]
                Gt = T("Gt", [128, 8, 16])
                M8 = T("M8", [128, 8, 8])
                CQ = T("CQ", [128, 8, 16])
                slq = T("slq", [128, 8])
                Pb = [T("Pb%d" % i, [128, 2, 256], BF16) for i in range(4)]
                PT = [T("PT%d" % i, [128, 4, 128], BF16) for i in range(4)]
                RS = T("RS", [128, 8, 17])
                lsum = T("lsum", [128, 8])
                Ofin = T("Ofin", [128, 8, 64], BF16)
                YB = T("YB", [128, 4, TB], BF16)
                mctr = [0]

                def moba_block(blk):
                    mb, half = blk // 2, blk % 2
                    q0 = blk * 128
                    nk = 128 * (half + 1)
                    pb = PSB[blk % 2]
                    for hp in range(4):
                        P.tr(pb[:, hp * 128:(hp + 1) * 128], VTf[:, hp, :], identb[:])
                    P.cp("dve", Vb[:, half, :, 0:64], pb[:, 0:512].rearrange("p (h d) -> p h d", h=8))
                    P.memset("pool", RS[:], 0.0)
                    P.ts("pool", slq[:], mbSl[:], float(-q0), ALU.mult)
                    if mb > 0:
                        pgs = (nps(), nps())
                        P.memset("pool", Gt[:], -1e30)
                        for h in range(8):
                            hp, e = h // 2, h % 2
                            P.mm(pgs[e][:, hp * 16:hp * 16 + mb], (QT32[e * 64:(e + 1) * 64, hp, :], hp),
                                 kmean[e * 64:(e + 1) * 64, hp, 0:mb])
                        for e in range(2):
                            P.cp("dve", Gt[:, e:8:2, 0:mb], pgs[e][:, 0:64].rearrange("p (a n) -> p a n", a=4)[:, :, 0:mb])
                        for h in range(8):
                            P.op("dve", lambda e_, h=h: e_.max(out=M8[:, h, :], in_=Gt[:, h, :]), (Gt.name,), (M8.name,))
                        for h in range(8):
                            P.ts("dve", CQ[:, h, :], Gt[:, h, :], M8[:, h, 2:3], ALU.is_lt, -30000.0, ALU.mult)
                        P.memset("dve", CQ[:, :, mb:mb + 1], 0.0)
                        P.tt("dve", CQ[:], CQ[:], mbT1[:], ALU.add)
                    else:
                        P.cp("dve", CQ[:], mbT1[:])
                    P.tt("dve", CQ[:], CQ[:], slq[:].unsqueeze(2).to_broadcast([128, 8, 16]), ALU.add)
                    yield
                    po = PSF[4]
                    pov = po[:].rearrange("p (h d) -> p h d", h=8)
                    first = [True]
                    nlast = [0]
                    loaded = {}

                    def kv(n):
                        if n == mb:
                            return KTb, Vb, nk, True
                        if n not in loaded:
                            kt_, v_ = KTs[n % 2], Vs[n % 2]
                            P.dma("sp", kt_[:], (KTd[n], "ktd%d" % n))
                            P.dma("sp", v_[:].rearrange("p a h d -> p a (h d)"), (Vd[n], "vd%d" % n))
                            loaded[n] = (kt_, v_)
                        kt_, v_ = loaded[n]
                        return kt_, v_, 256, False

                    def heads(hps):
                        for n in range(mb + 1):
                            kt_, v_, ncols, own = kv(n)
                            for hp in hps:
                                pse = (nps(), nps())
                                pbuf = Pb[hp]
                                for e in range(2):
                                    h = 2 * hp + e
                                    pv_ = pse[e]
                                    P.mm(pv_[:, 0:ncols], (QTb[e * 64:(e + 1) * 64, hp, :], hp),
                                         ((kt_[e * 64:(e + 1) * 64, hp, 0:ncols], hp) if own else kt_[e * 64:(e + 1) * 64, hp, 0:ncols]), start=True, stop=False)
                                    P.mm(pv_[:, 0:ncols], ones2[e * 64:e * 64 + 2, :], ALR[e * 64:e * 64 + 2, h, 0:ncols], start=False, stop=(not own))
                                    if own:
                                        c0 = half * 128
                                        P.mm(pv_[:, c0:c0 + 128], identb[:], trib[:], start=False, stop=True)
                                for e in range(2):
                                    h = 2 * hp + e
                                    P.act(pbuf[:, e, 0:ncols], pse[e][:, 0:ncols], AF.Exp, bias=CQ[:, h, n:n + 1],
                                          accum=RS[:, h, n:n + 1])
                                yield
                                ptb = PSB[hp % 2]
                                nt = ncols // 128
                                for e in range(2):
                                    for kt in range(nt):
                                        P.tr(ptb[:, (e * 2 + kt) * 128:(e * 2 + kt + 1) * 128], pbuf[:, e, kt * 128:(kt + 1) * 128], identb[:])
                                ptile = PT[hp]
                                if nt == 2:
                                    P.cp("dve", ptile[:], ptb[:, 0:512].rearrange("p (a q) -> p a q", a=4))
                                else:
                                    P.cp("dve", ptile[:, 0:1, :], ptb[:, 0:128].rearrange("p (a q) -> p a q", a=1))
                                    P.cp("dve", ptile[:, 2:3, :], ptb[:, 256:384].rearrange("p (a q) -> p a q", a=1))
                                for e in range(2):
                                    h = 2 * hp + e
                                    for kt in range(nt):
                                        nlast[0] += 1
                                        P.mm(pov[:, h, :], ptile[:, e * 2 + kt, :], (v_[:, kt, h, 0:64], v_.name),
                                             start=first[0], stop=False, skip_group_check=True)
                                        first[0] = False
                                yield
                    subs = [heads((0, 1)), heads((2, 3))]
                    while subs:
                        for g_ in list(subs):
                            try:
                                next(g_)
                            except StopIteration:
                                subs.remove(g_)
                        yield
                    P.op("dve", lambda e_: e_.reduce_sum(out=lsum[:], in_=RS[:], axis=AX.X), (RS.name,), (lsum.name,))
                    P.op("dve", lambda e_: e_.reciprocal(out=lsum[:], in_=lsum[:]), (lsum.name,), (lsum.name,))
                    P.tt("dve", Ofin[:], pov, lsum[:].unsqueeze(2).to_broadcast([128, 8, 64]), ALU.mult)
                    pb2 = PSB[blk % 2]
                    of = Ofin[:].rearrange("p h d -> p (h d)")
                    for c in range(4):
                        P.tr(pb2[:, c * 128:(c + 1) * 128], of[:, c * 128:(c + 1) * 128], identb[:])
                    P.cp("act", YB[:], pb2[:, 0:512].rearrange("p (c t) -> p c t", c=4))
                    if half == 1:
                        P.dma("pool", (KTd[mb], "ktd%d" % mb), KTb[:])
                        P.dma("pool", (Vd[mb], "vd%d" % mb), Vb[:].rearrange("p a h d -> p a (h d)"))
                        P.tt("pool", kmean[:, :, mb], ksum[:, :, 2 * mb], ksum[:, :, 2 * mb + 1], ALU.add)
                        P.ts("pool", kmean[:, :, mb], kmean[:, :, mb], 1.0 / 256.0, ALU.mult)


                CDEC = math.exp(-0.5)
                rwv = T("rwv", [128, 5, 4])
                P.dma("sp", rwv[:], D["rw_vecs"][l])
                hrw = T("hrw", [128, 2, 4])
                P.ts("dve", hrw[:], rwv[:, 0:2, :], 0.5, ALU.mult)
                w2a2f = T("w2a2f", [128, 512])
                W2A2 = T("W2A2", [128, 512], BF16)
                P.dma("sp", w2a2f[:], D["rw_w2a2"][l])
                P.cp("dve", W2A2[:], w2a2f[:])
                G2 = T("G2", [128, 512], BF16)
                P.dma("sp", w2a2f[:], D["rw_g2"][l])
                P.cp("dve", G2[:], w2a2f[:])
                lnwb = T("lnwb", [64, 2, 512])
                P.dma("sp", lnwb[:], D["rw_ln"][l])
                ST = T("ST", [64, 8, 64])
                P.memset("pool", ST[:], 0.0)
                LW = T("LW", [128, TB], BF16)
                SGx = T("SGx", [128, TB], BF16)
                F1 = [T("rwf%d" % i, [128, 4, TB]) for i in range(6)]
                F1x = T("rwfx", [128, 4, TB])
                Bq = {n: T("rwb_" + n, [128, 4, TB], BF16) for n in ("At", "Rt", "Bt", "Kt", "Bh", "Kh", "Vf", "Rk")}
                WCf = T("WCf", [128, 4, 2])
                WCt = T("WCt", [64, 4, 2, 2])
                TMc = T("TMc", [64, 5, 4, 128], BF16)
                SC = {n: T("rws_" + n, [64, 8, 64], BF16) for n in ("P", "PT", "Pn", "PnT", "ArbT", "LakT", "ArkT")}
                Zb = [T("rwZ%d" % i, [64, 8, 128], BF16) for i in range(2)]
                MT = T("rwMT", [64, 8, 64])
                DW = T("rwDW", [64, 8, 64])
                RpT = T("rwRpT", [64, 8, 64])
                Y32 = [T("rwY%d" % i, [64, 8, 64]) for i in range(2)]
                st1 = T("rwst1", [64, 8])
                st2 = T("rwst2", [64, 8])
                bsc = T("rwbs", [64, 8])
                YAt = T("YAt", [64, 512], BF16)
                YA = T("YA", [128, 4, TB], BF16)

                def bc4(col):
                    return col.unsqueeze(2).to_broadcast([128, 4, TB])

                def rwkv_block(blk):
                    r_, k_, v_ = RW[:, 0:4, :], RW[:, 4:8, :], RW[:, 8:12, :]
                    f0, f1, f2, f3, f4, f5 = F1
                    yield
                    P.act(LW[0:64, :], (RW[0:64, 12, :], 12), AF.Tanh)
                    P.cp("pool", LW[64:128, :], (RW[64:128, 12, :], 12))
                    P.act(SGx[:], (RW[:, 13, :], 13), AF.Tanh, scale=0.5)
                    P.ts("pool", SGx[:], SGx[:], 0.5, ALU.mult, 0.5, ALU.add)
                    pw = nps()
                    pa = nps()
                    pwv = pw[:].rearrange("p (a t) -> p a t", a=4)
                    pav = pa[:].rearrange("p (a t) -> p a t", a=4)
                    for hp in range(4):
                        P.mm(pwv[:, hp, :], W2A2[0:64, hp * 128:(hp + 1) * 128], LW[0:64, :])
                        P.mm(pav[:, hp, :], W2A2[64:128, hp * 128:(hp + 1) * 128], LW[64:128, :])
                    sgw, ai = f0, f1
                    for hp in range(4):
                        P.act(sgw[:, hp, :], pwv[:, hp, :], AF.Tanh, bias=hrw[:, 0, hp:hp + 1], scale=0.5)
                        P.act(ai[:, hp, :], pav[:, hp, :], AF.Tanh, bias=hrw[:, 1, hp:hp + 1], scale=0.5)
                    P.ts("pool", sgw[:], sgw[:], 0.5, ALU.mult, 0.5, ALU.add)
                    P.ts("pool", ai[:], ai[:], 0.5, ALU.mult, 0.5, ALU.add)
                    yield
                    kkr = f2
                    P.tt("pool", kkr[:], k_, bc4(rwv[:, 2, :]), ALU.mult)
                    sqb = Bq["Rk"]
                    P.tt("pool", sqb[:], kkr[:], kkr[:], ALU.mult)
                    pss = nps()
                    P.mm(pss[:], BOb[:], sqb[:].rearrange("p a t -> p (a t)"))
                    rn = f3
                    rnf = rn[:].rearrange("p a t -> p (a t)")
                    P.act(rnf, pss[:], AF.Sqrt)
                    P.ts("dve", rnf, rnf, 1e-12, ALU.max)
                    P.op("dve", lambda e_: e_.reciprocal(out=rnf, in_=rnf), (rn.name,), (rn.name,))
                    P.tt("pool", kkr[:], kkr[:], rn[:], ALU.mult)
                    yield
                    km = f3
                    P.stt("dve", km[:], ai[:], -1.0, bc4(rwv[:, 3, :]), ALU.add, ALU.mult)
                    P.ts("dve", km[:], km[:], 1.0, ALU.add)
                    P.tt("dve", km[:], km[:], k_, ALU.mult)
                    yield
                    P.tt("pool", f4[:], r_, bc4(rwv[:, 4, :]), ALU.mult)
                    P.tt("pool", Bq["Rk"][:], f4[:], km[:], ALU.mult)
                    yield
                    cs = f4
                    for hp in range(4):
                        for c in range(2):
                            P.scan(cs[:, hp, c * 64:(c + 1) * 64], ones64[:, 0:64], sgw[:, hp, c * 64:(c + 1) * 64], 0.0)
                    bkk = f5
                    P.tt("pool", bkk[:], kkr[:], ai[:], ALU.mult)
                    yield
                    ep, em = f1, f0
                    dprev = F1x
                    P.tt("dve", dprev[:], cs[:], sgw[:], ALU.subtract)
                    P.act(dprev[:], dprev[:], AF.Exp, scale=-CDEC)
                    P.stt("dve", Bq["At"][:], kkr[:], -1.0, dprev[:], ALU.mult, ALU.mult)
                    P.act(ep[:], cs[:], AF.Exp, scale=-CDEC)
                    P.tt("pool", Bq["Rt"][:], r_, ep[:], ALU.mult)
                    P.act(em[:], cs[:], AF.Exp, scale=CDEC)
                    P.tt("dve", Bq["Bt"][:], bkk[:], em[:], ALU.mult)
                    P.tt("pool", Bq["Kt"][:], km[:], em[:], ALU.mult)
                    yield
                    csv = cs[:].rearrange("p a (c t) -> p a c t", c=2)
                    dv = dprev[:].rearrange("p a (c t) -> p a c t", c=2)
                    P.tt("dve", dv, csv, csv[:, :, :, 63:64].to_broadcast([128, 4, 2, 64]), ALU.subtract)
                    P.act(dprev[:], dprev[:], AF.Exp, scale=CDEC)
                    P.tt("dve", Bq["Bh"][:], bkk[:], dprev[:], ALU.mult)
                    P.tt("pool", Bq["Kh"][:], km[:], dprev[:], ALU.mult)
                    P.act(WCf[:], csv[:, :, :, 63], AF.Exp, scale=-CDEC)
                    P.cp("pool", Bq["Vf"][:], v_)
                    yield
                    pwc = nps()
                    P.mm(pwc[0:64, 0:8], identf[:, 64:128], WCf[:].rearrange("p a c -> p (a c)"))
                    P.cp("dve", WCt[:, :, 0, :], WCf[0:64, :, :])
                    P.cp("dve", WCt[:, :, 1, :], pwc[0:64, 0:8].rearrange("p (a c) -> p a c", a=4))
                    for c in range(2):
                        if RWCUT > 0:
                            yield from rwkv_chunk(blk, c)

                def rwkv_chunk(blk, c):
                    tc = slice(c * 64, (c + 1) * 64)
                    yield
                    srcs = [Bq["At"], Bq["Bh"], Bq["Kh"], Bq["Vf"], Bq["Rt"]]
                    n = 0
                    for gi_, grp in enumerate(((0, 1), (2, 3), (4,))):
                        pb = PSB[(c + gi_) % 2]
                        for a_, si in enumerate(grp):
                            for hp in range(4):
                                col = (a_ * 4 + hp) * 128
                                P.tr(pb[0:64, col:col + 128], srcs[si][:, hp, tc], identb[:])
                        na = len(grp)
                        P.cp("act" if gi_ % 2 else "dve", TMc[:, grp[0]:grp[0] + na, :, :],
                             pb[0:64, 0:na * 512].rearrange("p (a h x) -> p a h x", a=na, h=4))

                    def tm(i, h):
                        return TMc[:, i, h // 2, (h % 2) * 64:(h % 2) * 64 + 64]

                    def fmr(name, h):
                        return Bq[name][(h % 2) * 64:(h % 2) * 64 + 64, h // 2, tc]
                    if RWCUT < 2:
                        return
                    yield
                    def score(dst, lname, rname, mi):
                        banks = (nps(), nps())
                        for h in range(8):
                            P.mm(banks[h % 2][0:64, (h // 2) * 64:(h // 2) * 64 + 64], fmr(lname, h), fmr(rname, h))
                        for e in range(2):
                            P.tt("dve", SC[dst][:, e:8:2, :], banks[e][0:64, 0:256].rearrange("p (a s) -> p a s", a=4),
                                 rwmask[:, mi:mi + 1, :].to_broadcast([64, 4, 64]), ALU.mult)
                    score("P", "At", "Bt", 0)
                    score("PT", "Bt", "At", 1)
                    score("LakT", "Kt", "At", 1)
                    score("ArbT", "Bt", "Rt", 2)
                    score("ArkT", "Kt", "Rt", 2)
                    if RWCUT < 3:
                        return
                    yield
                    pz = nps()
                    pzv = pz[0:64, :].rearrange("p (h s) -> p h s", h=8)
                    for h in range(8):
                        P.mm(pzv[:, h, :], SC["LakT"][:, h, :], tm(3, h))
                    Z = Zb[0]
                    P.cp("act", Z[:, :, 64:128], pzv)
                    P.cp("pool", Z[:, :, 0:64], TMc[:, 0, :, :].rearrange("p a (e x) -> p (a e) x", e=2))
                    if RWCUT < 4:
                        return
                    yield
                    Pk, PkT, Pn, PnT = SC["P"], SC["PT"], SC["Pn"], SC["PnT"]
                    for k in range(6):
                        Zn = Zb[(k + 1) % 2]
                        for hh in range(2):
                            pzz = nps()
                            pzzv = pzz[0:64, :].rearrange("p (h s) -> p h s", h=4)
                            for h4 in range(4):
                                h = hh * 4 + h4
                                P.mm(pzzv[:, h4, :], PkT[:, h, :], Z[:, h, :], start=(h4 == 0), stop=True, skip_group_check=True)
                            P.tt("dve", Zn[:, hh * 4:(hh + 1) * 4, :], pzzv, Z[:, hh * 4:(hh + 1) * 4, :], ALU.add)
                        Z = Zn
                        if k < 5:
                            ppT = nps()
                            ppTv = ppT[0:64, :].rearrange("p (h s) -> p h s", h=8)
                            for h in range(8):
                                P.mm(ppTv[:, h, :], Pk[:, h, :], PkT[:, h, :])
                            P.cp("dve", PnT[:], ppTv)
                        if k < 4:
                            pp = nps()
                            ppv = pp[0:64, :].rearrange("p (h s) -> p h s", h=8)
                            for h in range(8):
                                P.mm(ppv[:, h, :], PkT[:, h, :], Pk[:, h, :])
                            P.cp("act", Pn[:], ppv)
                        Pk, PkT, Pn, PnT = Pn, PnT, Pk, PkT
                        yield
                    if RWCUT < 5:
                        return
                    yield
                    yield
                    pM = nps()
                    pMv = pM[0:64, :].rearrange("p (h s) -> p h s", h=8)
                    for h in range(8):
                        P.mm(pMv[:, h, :], Z[:, h, 0:64], tm(1, h))
                    wcv = WCt[:, :, :, c].rearrange("p a e -> p (a e)")
                    P.tt("pool", DW[:], identf[0:64, 0:64].unsqueeze(1).to_broadcast([64, 8, 64]),
                         wcv.unsqueeze(2).to_broadcast([64, 8, 64]), ALU.mult)
                    P.tt("dve", MT[:], pMv, DW[:], ALU.add)
                    yield
                    pR = nps()
                    pRv = pR[0:64, :].rearrange("p (h s) -> p h s", h=8)
                    for h in range(8):
                        P.mm(pRv[:, h, :], Z[:, h, 0:64], SC["ArbT"][:, h, :], start=(h == 0), stop=False, skip_group_check=True)
                        P.mm(pRv[:, h, :], tm(4, h), identb[0:64, 0:64], start=False, stop=True, skip_group_check=True)
                    P.cp("act", RpT[:], pRv)
                    if RWCUT < 6:
                        return
                    yield
                    pY = nps()
                    pYv = pY[0:64, :].rearrange("p (h s) -> p h s", h=8)
                    for h in range(8):
                        P.mm(pYv[:, h, :], RpT[:, h, :], ST[:, h, :], start=(h == 0), stop=False, skip_group_check=True)
                        P.mm(pYv[:, h, :], SC["ArbT"][:, h, :], Z[:, h, 64:128], start=False, stop=False, skip_group_check=True)
                        P.mm(pYv[:, h, :], SC["ArkT"][:, h, :], tm(3, h), start=False, stop=True, skip_group_check=True)
                    y = Y32[c % 2]
                    P.cp("act", y[:], pYv)
                    yield
                    pS = PSF[5]
                    pSv = pS[0:64, :].rearrange("p (h s) -> p h s", h=8)
                    for h in range(8):
                        P.mm(pSv[:, h, :], MT[:, h, :], ST[:, h, :], start=(h == 0), stop=False, skip_group_check=True)
                        P.mm(pSv[:, h, :], tm(1, h), Z[:, h, 64:128], start=False, stop=False, skip_group_check=True)
                        P.mm(pSv[:, h, :], tm(2, h), tm(3, h), start=False, stop=True, skip_group_check=True)
                    P.cp("dve", ST[:], pSv)
                    if RWCUT < 7:
                        return
                    yield
                    sq = Y32[(c + 1) % 2]
                    P.op("dve", lambda e_: e_.reduce_sum(out=st1[:], in_=y[:], axis=AX.X), (y.name,), (st1.name,))
                    P.tt("pool", sq[:], y[:], y[:], ALU.mult)
                    P.op("dve", lambda e_: e_.reduce_sum(out=st2[:], in_=sq[:], axis=AX.X), (sq.name,), (st2.name,))
                    P.ts("dve", st1[:], st1[:], 1.0 / 64, ALU.mult)
                    P.ts("dve", st2[:], st2[:], 1.0 / 64, ALU.mult)
                    P.tt("dve", bsc[:], st1[:], st1[:], ALU.mult)
                    P.tt("dve", st2[:], st2[:], bsc[:], ALU.subtract)
                    P.ts("dve", st2[:], st2[:], 64e-5, ALU.add)
                    P.act(st2[:], st2[:], AF.Sqrt)
                    P.op("dve", lambda e_: e_.reciprocal(out=st2[:], in_=st2[:]), (st2.name,), (st2.name,))
                    P.tt("pool", y[:], y[:], st1[:].unsqueeze(2).to_broadcast([64, 8, 64]), ALU.subtract)
                    P.tt("pool", y[:], y[:], st2[:].unsqueeze(2).to_broadcast([64, 8, 64]), ALU.mult)
                    yf = y[:].rearrange("p h i -> p (h i)")
                    P.tt("pool", yf, yf, lnwb[:, 0, :], ALU.mult)
                    P.tt("pool", yf, yf, lnwb[:, 1, :], ALU.add)
                    yield
                    pbs = nps()
                    for hp in range(4):
                        P.mm(pbs[0:64, hp * 2:hp * 2 + 2], Bq["Rk"][:, hp, tc], BOb[:, 0:128:64])
                    P.cp("dve", bsc[:], pbs[0:64, 0:8])
                    vtm = TMc[:, 3, :, :].rearrange("p a (e x) -> p (a e) x", e=2)
                    P.tt("pool", sq[:], vtm, bsc[:].unsqueeze(2).to_broadcast([64, 8, 64]), ALU.mult)
                    P.tt("pool", y[:], y[:], sq[:], ALU.add)
                    yield
                    pG = nps()
                    P.mm(pG[0:64, :], SGx[:, tc], G2[:])
                    P.tt("dve", YAt[:], yf, pG[0:64, :], ALU.mult)
                    pb = PSB[c % 2]
                    for cc in range(4):
                        P.tr(pb[:, cc * 64:(cc + 1) * 64], YAt[:, cc * 128:(cc + 1) * 128], identb[0:64, 0:64])
                    P.cp("act", YA[:, :, tc], pb[:, 0:256].rearrange("p (a t) -> p a t", a=4))


                lnw1 = T("lnw1", [128, 1024])
                lnb1 = T("lnb1", [128, 1024])
                P.dma("sp", lnw1[:], D["ln1_rep"][l, 0])
                P.dma("sp", lnb1[:], D["ln1_rep"][l, 1])
                Hs = T("Hs", [128, 1024])
                xr = T("xr", [128, 1024])
                MG = T("MG", [128, 8, TB], BF16)
                lst = T("lst", [128, 4])
                XRES = D["x"] if l == 0 else X2

                def merge_block(blk):
                    t0 = blk * TB
                    P.dma("sp", xr[:], (XRES[t0:t0 + TB, :], "xres_%d" % blk))
                    hsv = Hs[:].rearrange("p (a t) -> p a t", a=8)
                    for half in range(2):
                        pus = []
                        for br, Y in enumerate((YA, YB, YC)):
                            wb = nwb()
                            wv = wb[:].rearrange("p a b -> p (a b)").rearrange("p (kc c) -> p kc c", kc=4)
                            P.dma("sp", (wv, wb.name), (UPB[l, br].rearrange("(kc p) c -> p kc c", p=128)[:, :, half * 512:(half + 1) * 512], "upb%d" % l))
                            pu = nps()
                            puv = pu[:].rearrange("p (a t) -> p a t", a=4)
                            for d4 in range(4):
                                for kc in range(4):
                                    P.mm(puv[:, d4, :], (wv[:, kc, d4 * 128:(d4 + 1) * 128], wb.name), Y[:, kc, :],
                                         start=(kc == 0 and d4 == 0), stop=(kc == 3), skip_group_check=True)
                            pus.append(puv)
                        a0_, a1_ = hsv[:, 0:4, :], hsv[:, 4:8, :]
                        g0 = half * 4
                        P.stt("dve", a0_, GS[:, g0:g0 + 4, :], 1.0, pus[0], ALU.add, ALU.mult)
                        P.stt("dve", a1_, GS[:, 8 + g0:8 + g0 + 4, :], 1.0, pus[1], ALU.add, ALU.mult)
                        P.tt("pool", a0_, a0_, a1_, ALU.add)
                        P.stt("dve", a1_, GS[:, 16 + g0:16 + g0 + 4, :], 1.0, pus[2], ALU.add, ALU.mult)
                        P.tt("pool", MG[:, g0:g0 + 4, :], a0_, a1_, ALU.add)
                    for cg in range(4):
                        wb = nwb()
                        P.dma("sp", wb[:], (WOB[l].rearrange("(kc p) c -> p kc c", p=128)[:, :, cg * 256:(cg + 1) * 256], "wob%d" % l))
                        ph = nps()
                        for kc in range(8):
                            P.mm(ph[:, 0:256], MG[:, kc, :], wb[:, kc, :], start=(kc == 0), stop=(kc == 7))
                        P.stt("dve", Hs[:, cg * 256:(cg + 1) * 256], xr[:, cg * 256:(cg + 1) * 256], float(2 * ALPHA), ph[:, 0:256], ALU.mult, ALU.add)
                    layer_norm(Hs, xr, lst, lnw1, lnb1, eps=4e-5)
                    P.dma("pool", (X1[t0:t0 + TB, :], "x1_%d" % blk), Hs[:])

                def layer_norm(h_, junk, st_, w_, b_, eps=1e-5):
                    P.act(junk[:], h_[:], AF.Copy, accum=st_[:, 0:1])
                    P.act(junk[:], h_[:], AF.Square, accum=st_[:, 1:2])
                    P.ts("dve", st_[:, 0:2], st_[:, 0:2], 1.0 / DM, ALU.mult)
                    P.tt("dve", st_[:, 2:3], st_[:, 0:1], st_[:, 0:1], ALU.mult)
                    P.tt("dve", st_[:, 1:2], st_[:, 1:2], st_[:, 2:3], ALU.subtract)
                    P.ts("dve", st_[:, 1:2], st_[:, 1:2], float(eps), ALU.add)
                    P.act(st_[:, 1:2], st_[:, 1:2], AF.Sqrt)
                    P.op("dve", lambda e_: e_.reciprocal(out=st_[:, 1:2], in_=st_[:, 1:2]), (st_.name,), (st_.name,))
                    P.ts("dve", h_[:], h_[:], st_[:, 0:1], ALU.subtract, st_[:, 1:2], ALU.mult)
                    P.tt("dve", h_[:], h_[:], w_[:], ALU.mult)
                    P.tt("dve", h_[:], h_[:], b_[:], ALU.add)

                ngrp = 27
                xtb = [T("xtb%d" % i, [128, 8, TB + 1], BF16) for i in range(2)]
                def inproj(blk, xt, g0, g1):
                    for g in range(g0, g1):
                        wb = nwb()
                        ncol = 256
                        P.dma("sp", (wb[:, :, 0:ncol], wb.name), (WV[:, :, g * 256: g * 256 + ncol], "winb%d" % l))
                        for mm_ in range(ncol // 128):
                            m = g * 2 + mm_
                            ps = nps()
                            shift = m < 14
                            n = TB + 1 if shift else TB
                            c0 = 0 if shift else 1
                            for kc in range(8):
                                P.mm(ps[:, 0:n], (wb[:, kc, mm_ * 128:(mm_ + 1) * 128], wb.name),
                                     xt[:, kc, c0:c0 + n], start=(kc == 0), stop=(kc == 7))
                            if shift:
                                ta = tmpA[m % 2]
                                P.act(ta[:], ps[:, 1:TB + 1], AF.Copy, scale=omu[:, m:m + 1])
                                P.stt("dve", (RW[:, m, :], m), ps[:, 0:TB], mu[:, m:m + 1], ta[:], ALU.mult, ALU.add)
                            elif m < 18:
                                P.act((QT32[:, m - 14, :], m - 14), ps[:, 0:TB], AF.Copy, scale=0.125)
                                P.cp("pool", (QTb[:, m - 14, :], m - 14), (QT32[:, m - 14, :], m - 14))
                            elif m < 22:
                                hf = blk % 2
                                P.act((KTb[:, m - 18, hf * 128:(hf + 1) * 128], m - 18), ps[:, 0:TB], AF.Copy,
                                      accum=ksum[:, m - 18, blk:blk + 1])
                            elif m < 26:
                                P.act((VTf[:, m - 22, :], m - 22), ps[:, 0:TB], AF.Copy)
                            elif m < 30:
                                P.act((U32[:, m - 26, :], m - 26), ps[:, 0:TB], AF.Copy)
                                P.cp("pool", (Ub[:, m - 26, :], m - 26), (U32[:, m - 26, :], m - 26))
                            else:
                                P.act((GS[:, m - 30, :], m - 30), ps[:, 0:TB], AF.Tanh, bias=hgateb[:, m - 30:m - 29], scale=0.5)
                        yield

                def load_xt(blk):
                    xt_ = xtb[blk % 2]
                    tt0 = blk * TB
                    P.dma_fn("sp", lambda e, xt_=xt_, tt0=tt0: e.dma_start(out=xt_[:], in_=XTd[:, :, tt0:tt0 + TB + 1]),
                             ("XT_%d" % blk, ("XT_%d" % (blk - 1)) if blk else "XTz"), (xt_.name,))

                load_xt(0)
                for blk in range(nblk):
                    t0 = blk * TB
                    xt = xtb[blk % 2]
                    for _ in inproj(blk, xt, 0, 15):
                        pass
                    if blk + 1 < nblk:
                        load_xt(blk + 1)
                    gens = [inproj(blk, xt, 15, 27)]
                    if 'rwkv' not in SKIP:
                        gens.append(rwkv_block(blk))
                    if 'moba' not in SKIP:
                        gens.append(moba_block(blk))
                    if 's5blk' not in SKIP and 's5' not in SKIP:
                        gens.append(s5_block(blk))
                    gate_gen = gens[0]
                    rnd = 0
                    while gens:
                        for g_ in list(gens):
                            reps = 1
                            for _r in range(reps):
                                try:
                                    next(g_)
                                except StopIteration:
                                    if g_ in gens:
                                        gens.remove(g_)
                                    break
                        rnd += 1
                    if 'merge' not in SKIP:
                        merge_block(blk)
                    if debug and blk == dbg_blk and l == 0:
                        d32 = T("d32", [128, 4, TB])
                        if "X1" in dbg and "merge" not in SKIP:
                            P.dma("sp", dbg["X1"], Hs[:])
                        if "YA" in dbg and "rwkv" not in SKIP and RWCUT >= 7:
                            P.cp("dve", d32[:, 0:4, :], YA[:])
                            P.dma("sp", dbg["YA"], d32[:, 0:4, :])
                        if "YB" in dbg and "moba" not in SKIP:
                            P.cp("dve", d32[:, 0:4, :], YB[:])
                            P.dma("sp", dbg["YB"], d32[:, 0:4, :])
                        if "YC" in dbg:
                            P.cp("dve", d32[:, 0:4, :], YC[:])
                            P.dma("sp", dbg["YC"], d32[:, 0:4, :])
                print('sbuf remaining (mixer phase)', nc.sbuf_bytes_remaining)
                P.barrier(bar[:])

            if 'moe' in SKIP:
                continue
            with ExitStack() as se:
                def T(name, shape, dt=F32):
                    return P.sb(name, shape, dt, se)
                last = (l == n_layers - 1)
                NT = nblk
                NB = 2 * NT + 32
                rwt = T("m_rw", [128, 8, 36])
                rbt = T("m_rb", [128, 36])
                trib_s = T("m_tri", [128, 128], BF16)
                onesb = T("m_ones", [128, 128], BF16)
                thr = T("m_thr", [128, 64])
                nv = T("m_nv", [128, 96])
                pvt = T("m_pv", [128, 1])
                lnw2 = T("lnw2", [128, 1024])
                lnb2 = T("lnb2", [128, 1024])
                tmp32 = T("m_tmp32", [128, 128])
                P.dma("sp", rwt[:], D["moe_rw"][l])
                P.dma("sp", rbt[:], D["moe_rb"][l])
                P.dma("sp", tmp32[:], D["moe_tri"][:, :])
                P.cp("dve", trib_s[:], tmp32[:])
                P.memset("pool", onesb[:], 1.0)
                P.dma("sp", thr[:], D["moe_thr"][:, :])
                P.dma("sp", nv[:], D["moe_nv"][:, :])
                P.dma("sp", pvt[:], D["moe_pv"][:, :])
                P.dma("sp", lnw2[:], D["ln2_rep"][l, 0])
                P.dma("sp", lnb2[:], D["ln2_rep"][l, 1])
                OHK = T("m_ohk", [128, NT, 2, 32])
                OHS = T("m_ohs", [128, NT, 32], BF16)
                PF = T("m_pf", [128, NT, 32])
                WT = T("m_wt", [128, NT, 2])
                cnt = T("m_cnt", [128, 32])
                P.memset("pool", cnt[:], 0.0)
                xt32 = [T("m_x%d" % i, [128, 1024]) for i in range(2)]
                xT32 = T("m_xT", [128, 8, 128])
                LG = T("m_lg", [128, 36])
                sm_ = {n: T("m_s_" + n, [128, 8]) for n in ("a", "b", "c", "d", "m8")}
                g4 = {n: T("m_g_" + n, [128, 4]) for n in ("oh", "e", "t")}
                e32 = T("m_e32", [128, 4, 8])
                for ti in range(NT):
                    x_ = xt32[ti % 2]
                    P.dma("sp", x_[:], (X1[ti * 128:(ti + 1) * 128, :], "x1_%d" % ti))
                    for hh in range(2):
                        pt = nps()
                        for k4 in range(4):
                            kc = hh * 4 + k4
                            P.tr(pt[:, k4 * 128:(k4 + 1) * 128], x_[:, kc * 128:(kc + 1) * 128], identf[:])
                        P.cp("act" if hh else "dve", xT32[:, hh * 4:(hh + 1) * 4, :], pt[:].rearrange("p (a t) -> p a t", a=4))
                    pl = nps()
                    for kc in range(8):
                        P.mm(pl[:, 0:36], xT32[:, kc, :], rwt[:, kc, :], start=(kc == 0), stop=(kc == 7))
                    P.tt("dve", LG[:], pl[:, 0:36], rbt[:], ALU.add)
                    a_, b_, c_, d_, m8 = sm_["a"], sm_["b"], sm_["c"], sm_["d"], sm_["m8"]
                    P.op("dve", lambda e_: e_.reduce_max(out=a_[:, 0:1], in_=LG[:, 0:4], axis=AX.X), (LG.name,), (a_.name,))
                    P.ts("dve", g4["oh"][:], LG[:, 0:4], a_[:, 0:1], ALU.is_equal)
                    P.ts("dve", a_[:, 1:2], a_[:, 0:1], -1.0, ALU.mult)
                    P.act(g4["e"][:], LG[:, 0:4], AF.Exp, bias=a_[:, 1:2], accum=a_[:, 2:3])
                    P.op("dve", lambda e_: e_.reciprocal(out=a_[:, 3:4], in_=a_[:, 2:3]), (a_.name,), (a_.name,))
                    P.tt("dve", e32[:], LG[:, 4:36].rearrange("p (g e) -> p g e", g=4),
                         g4["oh"][:].unsqueeze(2).to_broadcast([128, 4, 8]), ALU.mult)
                    P.op("dve", lambda e_: e_.reduce_sum(out=b_[:], in_=e32[:].rearrange("p g e -> p e g"), axis=AX.X),
                         (e32.name,), (b_.name,))
                    P.op("dve", lambda e_: e_.max(out=m8[:], in_=b_[:]), (b_.name,), (m8.name,))
                    P.tt("dve", c_[:, 0:1], m8[:, 1:2], m8[:, 0:1], ALU.subtract)
                    P.act(c_[:, 1:2], c_[:, 0:1], AF.Exp)
                    P.ts("dve", c_[:, 1:2], c_[:, 1:2], 1.0, ALU.add)
                    P.op("dve", lambda e_: e_.reciprocal(out=c_[:, 2:3], in_=c_[:, 1:2]), (c_.name,), (c_.name,))
                    P.ts("dve", c_[:, 3:4], c_[:, 2:3], -1.0, ALU.mult, 1.0, ALU.add)
                    P.tt("dve", WT[:, ti, 0:1], c_[:, 2:3], a_[:, 3:4], ALU.mult)
                    P.tt("dve", WT[:, ti, 1:2], c_[:, 3:4], a_[:, 3:4], ALU.mult)
                    for k in range(2):
                        P.ts("dve", d_[:], b_[:], m8[:, k:k + 1], ALU.is_equal)
                        P.tt("dve", OHK[:, ti, k, :].rearrange("p (g e) -> p g e", g=4),
                             g4["oh"][:].unsqueeze(2).to_broadcast([128, 4, 8]),
                             d_[:].unsqueeze(1).to_broadcast([128, 4, 8]), ALU.mult)
                    P.tt("dve", OHS[:, ti, :], OHK[:, ti, 0, :], OHK[:, ti, 1, :], ALU.add)
                    pp_ = nps()
                    P.mm(pp_[:, 0:32], trib_s[:], OHS[:, ti, :])
                    P.tt("dve", PF[:, ti, :], pp_[:, 0:32], cnt[:], ALU.add)
                    pc_ = nps()
                    P.mm(pc_[:, 0:32], onesb[:], OHS[:, ti, :])
                    P.tt("dve", cnt[:], cnt[:], pc_[:, 0:32], ALU.add)
                cmpb = T("m_cmp", [128, 96, 32])
                nbk = T("m_nbk", [128, 32])
                pend = T("m_pend", [128, 32])
                pst = T("m_pst", [128, 32])
                ones32 = T("m_o32", [128, 32])
                P.memset("pool", ones32[:], 1.0)
                cv = cmpb[:, 0:64, :].rearrange("p k e -> p e k")
                P.tt("dve", cv, cnt[:].unsqueeze(2).to_broadcast([128, 32, 64]), thr[:].unsqueeze(1).to_broadcast([128, 32, 64]), ALU.is_gt)
                P.op("dve", lambda e_: e_.reduce_sum(out=nbk[:], in_=cv, axis=AX.X), (cmpb.name,), (nbk.name,))
                P.scan(pend[:], ones32[:], nbk[:], 0.0)
                P.tt("dve", pst[:], pend[:], nbk[:], ALU.subtract)
                P.ts("dve", pst[:], pst[:], 128.0, ALU.mult)
                P.ts("dve", pend[:], pend[:], 128.0, ALU.mult)
                P.tt("dve", cmpb[:], pend[:].unsqueeze(1).to_broadcast([128, 96, 32]), nv[:].unsqueeze(2).to_broadcast([128, 96, 32]), ALU.is_le)
                bke = T("m_bke", [128, 96])
                P.op("dve", lambda e_: e_.reduce_sum(out=bke[:], in_=cmpb[:], axis=AX.X), (cmpb.name,), (bke.name,))
                P.ts("dve", bke[:], bke[:], 31.0, ALU.min, 128.0, ALU.mult)
                P.ts("dve", bke[:], bke[:], pvt[:, 0:1], ALU.add)
                dup = T("m_dup", [128, 96])
                P.memset("pool", dup[:], 0.0)
                P.tt("dve", dup[:, 2:96], bke[:, 2:96], bke[:, 0:94], ALU.is_equal)
                P.ts("dve", dup[:], dup[:], 1.0e6, ALU.mult)
                P.tt("dve", bke[:], bke[:], dup[:], ALU.add)
                idxw = T("m_idxw", [128, 96], I32)
                P.cp("dve", idxw[:], bke[:])
                DEST = T("m_dest", [128, NT, 2])
                DESTi = T("m_desti", [128, NT, 2], I32)
                dtmp = T("m_dtmp", [128, 32])
                for ti in range(NT):
                    P.tt("dve", PF[:, ti, :], PF[:, ti, :], pst[:], ALU.add)
                    for k in range(2):
                        P.tt("dve", dtmp[:], PF[:, ti, :], OHK[:, ti, k, :], ALU.mult)
                        P.op("dve", lambda e_, ti=ti, k=k: e_.reduce_sum(out=DEST[:, ti, k:k + 1], in_=dtmp[:], axis=AX.X),
                             (dtmp.name,), (DEST.name,))
                P.cp("dve", DESTi[:], DEST[:])
                xb16 = [T("m_xb%d" % i, [128, 1024], BF16) for i in range(2)]
                for ti in range(NT):
                    x_ = xt32[ti % 2]
                    xb_ = xb16[ti % 2]
                    P.dma("sp", x_[:], (X1[ti * 128:(ti + 1) * 128, :], "x1_%d" % ti))
                    P.cp("act" if ti % 2 else "dve", xb_[:], x_[:])
                    for k in range(2):
                        P.scatter((XS[0:NB * 128, :], "xs"), xb_[:], DESTi[:, ti, k:k + 1])
                wgu_f = [T("m_wgu%d" % i, [128, 4096], BF16) for i in range(2)]
                wdt_f = [T("m_wd%d" % i, [128, 2048], BF16) for i in range(2)]
                wgu1 = [w_[:].rearrange("p (s k c) -> p s k c", s=2, k=8) for w_ in wgu_f]
                wdt1 = [w_[:].rearrange("p (f c) -> p f c", f=2) for w_ in wdt_f]
                xbT = [T("m_xbT%d" % i, [128, 8, 128], BF16) for i in range(2)]
                sgl = [T("m_sg%d" % i, [128, 2, 128], BF16) for i in range(2)]
                hid = [T("m_hid%d" % i, [128, 2, 128], BF16) for i in range(2)]
                yo = [T("m_yo%d" % i, [128, 1024]) for i in range(2)]
                WG2 = WGUB[l]
                WD2 = WDB[l]
                for n in range(NB):
                    j = n % 2
                    xb_ = xb16[j]
                    P.dma("sp", xb_[:], (XS[n * 128:(n + 1) * 128, :], "xs"))
                    P.gather(wgu_f[j][:], (WG2, "wgub%d" % l), idxw[:, n:n + 1], bound=4095)
                    P.gather(wdt_f[j][:], (WD2, "wdb%d" % l), idxw[:, n:n + 1], bound=4095)
                    pb = PSB[j]
                    for kc in range(8):
                        P.tr(pb[:, kc * 128:(kc + 1) * 128], xb_[:, kc * 128:(kc + 1) * 128], identb[:])
                    P.cp("dve", xbT[j][:], pb[:].rearrange("p (k t) -> p k t", k=8))
                    pgu = nps()
                    pguv = pgu[:].rearrange("p (a t) -> p a t", a=4)
                    for s_ in range(2):
                        for ft in range(2):
                            for kc in range(8):
                                P.mm(pguv[:, s_ * 2 + ft, :], (wgu1[j][:, s_, kc, ft * 128:(ft + 1) * 128], wgu_f[j].name), xbT[j][:, kc, :],
                                     start=(kc == 0 and s_ == 0 and ft == 0), stop=(kc == 7), skip_group_check=True)
                    P.act(sgl[j][:], pguv[:, 0:2, :], AF.Silu)
                    P.tt("dve", hid[j][:], pguv[:, 2:4, :], sgl[j][:], ALU.mult)
                    for hf in range(2):
                        py_ = nps()
                        for fc in range(2):
                            P.mm(py_[:], hid[j][:, fc, :], (wdt1[j][:, fc, hf * 512:(hf + 1) * 512], wdt_f[j].name), start=(fc == 0), stop=(fc == 1))
                        P.cp("act" if hf else "dve", yo[j][:, hf * 512:(hf + 1) * 512], py_[:])
                    P.dma("sp", (YBd[n * 128:(n + 1) * 128, :], "ybd"), yo[j][:])
                g01 = [T("m_g%d" % i, [128, 1024]) for i in range(4)]
                ljunk = T("m_ljunk", [128, 1024])
                lst2 = T("m_lst", [128, 4])
                for ti in range(NT):
                    x_ = xt32[ti % 2]
                    P.dma("sp", x_[:], (X1[ti * 128:(ti + 1) * 128, :], "x1_%d" % ti))
                    for k in range(2):
                        P.gather(g01[2 * (ti % 2) + k][:], (YBd[0:NB * 128, :], "ybd"), DESTi[:, ti, k:k + 1])
                    P.act(x_[:], x_[:], AF.Copy, scale=float(ALPHA))
                    P.stt("dve", x_[:], g01[2 * (ti % 2)][:], WT[:, ti, 0:1], x_[:], ALU.mult, ALU.add)
                    P.stt("dve", x_[:], g01[2 * (ti % 2) + 1][:], WT[:, ti, 1:2], x_[:], ALU.mult, ALU.add)
                    layer_norm(x_, ljunk, lst2, lnw2, lnb2)
                    if last:
                        P.dma("sp", (OUT[ti * 128:(ti + 1) * 128, :], "out"), x_[:])
                    else:
                        P.dma("sp", (X2[ti * 128:(ti + 1) * 128, :], "xres_%d" % ti), x_[:])
                        xb_ = xb16[ti % 2]
                        P.cp("act", xb_[:], x_[:])
                        pb = PSB[ti % 2]
                        for kc in range(8):
                            P.tr(pb[:, kc * 128:(kc + 1) * 128], xb_[:, kc * 128:(kc + 1) * 128], identb[:])
                        P.cp("dve", xbT[ti % 2][:], pb[:].rearrange("p (k t) -> p k t", k=8))
                        P.dma("sp", (XTd[:, :, 1 + ti * 128: 1 + (ti + 1) * 128], "XT_%d" % ti), xbT[ti % 2][:])
                    if debug and l == 0 and ti == dbg_blk and "X2" in dbg:
                        P.dma("sp", dbg["X2"], x_[:])
                P.barrier(bar[:])
        stats = P.finish()
        print("prog stats", stats)
    return nc


def kernel(**inputs):
    inp = {k: np.asarray(v) for k, v in inputs.items()}
    sh = prep_shared(inp)
    nc = bass.Bass("TRN2", target_bir_lowering=False)
    build(nc)
    shared = {k: np.ascontiguousarray(sh[k], dtype=np.float32) for k in IN_SHAPES if k != "x"}
    in_maps = []
    for c in range(8):
        m = dict(shared)
        m["x"] = np.ascontiguousarray(inp["x"][c % 4], dtype=np.float32)
        in_maps.append(m)
    res = run_bass_kernel_spmd(nc, in_maps, core_ids=list(range(8)))
    out = np.stack([np.asarray(res.results[b]["out"]) for b in range(4)]).astype(np.float32)
    return out
```
